# Optimizing a Trainium2 kernel written in Bass

```python
import jax, jax.numpy as jnp
from jax import lax
import numpy as np

D_MODEL = 1024
BATCH = 8
SEQ = 2048
DEPTH = 2

GRID_W = 64
CTX_LEN = 256
D_MIX = D_MODEL
A_HEADS = 4
A_DH = 64
A_DV = 2 * A_DH
A_WIDTH = A_HEADS * A_DV
M_HEADS = 4
M_WIDTH = D_MIX - A_WIDTH
M_DH = M_WIDTH // M_HEADS
CONV_K = 5
CHUNK = 64
Q_BLOCK = 128
ROPE_BASE = 10000.0
ROPE_PAIRS_PER_AXIS = A_DH // 4
D_FF = 256 * ((8 * D_MODEL // 3 + 255) // 256)
N_EXPERTS = 8
TOP_K = 2
D_FF_EXPERT = 7 * D_MODEL // 2
N_DENSE = (DEPTH + 1) // 2
N_MOE = DEPTH // 2
EPS = 1e-6
N_GATES = 4 * M_HEADS
IN_COLS = 3 * A_WIDTH + 4 * M_WIDTH + N_GATES
SPLITS = (A_WIDTH, 2 * A_WIDTH, 3 * A_WIDTH, 3 * A_WIDTH + M_WIDTH, 3 * A_WIDTH + 2 * M_WIDTH,
          3 * A_WIDTH + 3 * M_WIDTH, 3 * A_WIDTH + 4 * M_WIDTH)

kernel_name = "hybrid_diffattn_mlstm_moe_dit"


def rms_norm(x, g):
    xf = x.astype(jnp.float32)
    y = xf * lax.rsqrt(jnp.mean(xf * xf, axis=-1, keepdims=True) + EPS)
    return (y * g.astype(jnp.float32)).astype(x.dtype)


def adaln(cvec, w, b):
    m = jax.nn.silu(cvec) @ w + b
    return jnp.split(m[..., None, :], 6, axis=-1)


def modulate(x, g, shift, scale):
    return rms_norm(x, g) * (1.0 + scale) + shift


def axial_rope_tables(n_tokens):
    rows = n_tokens // GRID_W
    row = jnp.repeat(jnp.arange(rows, dtype=jnp.float32), GRID_W)
    col = jnp.tile(jnp.arange(GRID_W, dtype=jnp.float32), rows)
    inv = ROPE_BASE ** (-jnp.arange(ROPE_PAIRS_PER_AXIS, dtype=jnp.float32) / ROPE_PAIRS_PER_AXIS)
    ang = jnp.concatenate([row[:, None] * inv, col[:, None] * inv], axis=-1)
    return jnp.cos(ang), jnp.sin(ang)


def apply_rope(x, cos, sin):
    c = cos[None, :, None, None, :].astype(x.dtype)
    s = sin[None, :, None, None, :].astype(x.dtype)
    x1, x2 = x[..., :A_DH // 2], x[..., A_DH // 2:]
    return jnp.concatenate([x1 * c - x2 * s, x1 * s + x2 * c], axis=-1)


def diff_heads(aq, ak, av, q_g, k_g):
    B, T, _ = aq.shape
    q = rms_norm(aq.reshape(B, T, A_HEADS, 2, A_DH), q_g)
    k = rms_norm(ak.reshape(B, T, A_HEADS, 2, A_DH), k_g)
    v = av.reshape(B, T, A_HEADS, A_DV)
    return q, k, v


def diff_attention(q, k, v, lam):
    s = jnp.einsum('bqhcd,bkhcd->bhcqk', q, k).astype(jnp.float32) * (A_DH ** -0.5)
    p = jax.nn.softmax(s, axis=-1)
    a = p[:, :, 0] - lam * p[:, :, 1]
    return jnp.einsum('bhqk,bkhv->bqhv', a.astype(v.dtype), v)


def blocked_latent_attention(q, k_all, v_all, lam):
    B, T = q.shape[:2]
    nb = T // Q_BLOCK
    qb = jnp.moveaxis(q.reshape(B, nb, Q_BLOCK, A_HEADS, 2, A_DH), 1, 0)

    def one_block(blk):
        return diff_attention(blk, k_all, v_all, lam)

    out = lax.map(one_block, qb)
    return jnp.moveaxis(out, 0, 1).reshape(B, T, A_HEADS, A_DV)


def diff_post(o, g, lam_init):
    B, T = o.shape[:2]
    return (rms_norm(o, g) * (1.0 - lam_init).astype(o.dtype)).reshape(B, T, A_WIDTH)


def centred_dwconv(x, w, b):
    pad = CONV_K // 2
    y = lax.conv_general_dilated(x, w[:, None, :].astype(x.dtype), window_strides=(1,),
                                 padding=[(pad, pad)], dimension_numbers=('NWC', 'WIO', 'NWC'),
                                 feature_group_count=x.shape[-1])
    return y + b


def to_heads(a):
    B, T, _ = a.shape
    return a.reshape(B, T, M_HEADS, M_DH).transpose(0, 2, 1, 3).astype(jnp.float32)


def mlstm_inputs(mq, mk, mv, gates, conv_w, conv_b, gate_b):
    B, T, _ = mq.shape
    qk = jax.nn.silu(centred_dwconv(jnp.concatenate([mq, mk], axis=-1), conv_w, conv_b))
    q, k = jnp.split(qk, 2, axis=-1)
    g = (gates.reshape(B, T, 2, 2, M_HEADS) + gate_b).astype(jnp.float32).transpose(2, 3, 0, 4, 1)
    log_i = g[:, 0]
    log_f = jax.nn.log_sigmoid(g[:, 1])
    return to_heads(q), to_heads(k) * (M_DH ** -0.5), to_heads(mv), log_i, log_f


def zero_state(B):
    return (jnp.zeros((B, M_HEADS, M_DH, M_DH), jnp.float32),
            jnp.zeros((B, M_HEADS, M_DH), jnp.float32),
            jnp.zeros((B, M_HEADS), jnp.float32))


def mlstm_chunkwise(q, k, v, log_i, log_f, state):
    B, H, T, dk = q.shape
    dv = v.shape[-1]
    nc = T // CHUNK

    def to_chunks(a):
        return jnp.moveaxis(a.reshape(B, H, nc, CHUNK, *a.shape[3:]), 2, 0)

    causal = jnp.tril(jnp.ones((CHUNK, CHUNK), dtype=bool))

    def step(carry, inp):
        C, n, m = carry
        qb, kb, vb, li, lf = inp
        b = jnp.cumsum(lf, axis=-1)
        dmat = jnp.where(causal, b[..., :, None] - b[..., None, :] + li[..., None, :], -jnp.inf)
        inter = b + m[..., None]
        m_t = jnp.maximum(inter, jnp.max(dmat, axis=-1))
        w_intra = jnp.exp(dmat - m_t[..., None])
        w_inter = jnp.exp(inter - m_t)
        s = jnp.einsum('bhld,bhsd->bhls', qb, kb) * w_intra
        num = jnp.einsum('bhls,bhsv->bhlv', s, vb) + w_inter[..., None] * jnp.einsum('bhld,bhdv->bhlv', qb, C)
        den = jnp.sum(s, axis=-1) + w_inter * jnp.einsum('bhld,bhd->bhl', qb, n)
        h = num / jnp.maximum(jnp.abs(den), jnp.exp(-m_t))[..., None]
        b_last = b[..., -1]
        w_log = b_last[..., None] - b + li
        m_new = jnp.maximum(b_last + m, jnp.max(w_log, axis=-1))
        w = jnp.exp(w_log - m_new[..., None])
        decay = jnp.exp(b_last + m - m_new)
        C_new = decay[..., None, None] * C + jnp.einsum('bhsd,bhsv->bhdv', kb * w[..., None], vb)
        n_new = decay[..., None] * n + jnp.einsum('bhs,bhsd->bhd', w, kb)
        return (C_new, n_new, m_new), h

    state, hs = lax.scan(step, state, (to_chunks(q), to_chunks(k), to_chunks(v), to_chunks(log_i), to_chunks(log_f)))
    return jnp.moveaxis(hs, 0, 2).reshape(B, H, T, dv), state


def flip_t(a):
    return jnp.flip(a, axis=2)


def bidir_mlstm(q, k, v, log_i, log_f, init_f, init_b):
    h_f, st_f = mlstm_chunkwise(q, k, v, log_i[0], log_f[0], init_f)
    h_b, st_b = mlstm_chunkwise(flip_t(q), flip_t(k), flip_t(v), flip_t(log_i[1]), flip_t(log_f[1]), init_b)
    return h_f + flip_t(h_b), st_f, st_b


def mlstm_post(h, o, g):
    B, T, _ = o.shape
    hn = rms_norm(jnp.transpose(h, (0, 2, 1, 3)), g.reshape(M_HEADS, M_DH)).astype(o.dtype)
    return jax.nn.sigmoid(o) * hn.reshape(B, T, M_WIDTH)


def hybrid_mixer(h_lat, h_ctx, cos, sin, w_in_l, w_out_l, q_g, k_g, lam, lam_init, subln_g_l,
                 conv_w_l, conv_b_l, gate_b_l, mnorm_g_l, need_ctx):
    pl = jnp.split(h_lat @ w_in_l, SPLITS, axis=-1)
    pc = jnp.split(h_ctx @ w_in_l, SPLITS, axis=-1)
    ql, kl, vl = diff_heads(pl[0], pl[1], pl[2], q_g, k_g)
    ql, kl = apply_rope(ql, cos, sin), apply_rope(kl, cos, sin)
    qc, kc, vc = diff_heads(pc[0], pc[1], pc[2], q_g, k_g)
    k_all = jnp.concatenate([kl, kc], axis=1)
    v_all = jnp.concatenate([vl, vc], axis=1)
    a_lat = diff_post(blocked_latent_attention(ql, k_all, v_all, lam), subln_g_l, lam_init)
    mc = mlstm_inputs(pc[3], pc[4], pc[5], pc[7], conv_w_l, conv_b_l, gate_b_l)
    ml = mlstm_inputs(pl[3], pl[4], pl[5], pl[7], conv_w_l, conv_b_l, gate_b_l)
    z = zero_state(h_lat.shape[0])
    hc_sum, st_f, st_b = bidir_mlstm(mc[0], mc[1], mc[2], mc[3], mc[4], z, z)
    hl_sum, _, _ = bidir_mlstm(ml[0], ml[1], ml[2], ml[3], ml[4], st_f, st_b)
    m_lat = mlstm_post(hl_sum, pl[6], mnorm_g_l)
    out_lat = jnp.concatenate([a_lat, m_lat], axis=-1) @ w_out_l
    out_ctx = None
    if need_ctx:
        a_ctx = diff_post(diff_attention(qc, kc, vc, lam), subln_g_l, lam_init)
        m_ctx = mlstm_post(hc_sum, pc[6], mnorm_g_l)
        out_ctx = jnp.concatenate([a_ctx, m_ctx], axis=-1) @ w_out_l
    return out_lat, out_ctx


def swiglu(h, w1, w3, w2):
    return (jax.nn.silu(h @ w1) * (h @ w3)) @ w2


def moe_swiglu(h, w_r, w1, w3, w2):
    B, T, D = h.shape
    t = h.reshape(B * T, D)
    logits = (t @ w_r).astype(jnp.float32)
    top_val, top_idx = lax.top_k(logits, TOP_K)
    top_w = jax.nn.softmax(top_val, axis=-1)
    comb = jnp.sum(jax.nn.one_hot(top_idx, N_EXPERTS, dtype=jnp.float32) * top_w[..., None], axis=1).astype(h.dtype)
    out = jnp.zeros_like(t)
    for e in range(N_EXPERTS):
        out = out + comb[:, e:e + 1] * swiglu(t, w1[e], w3[e], w2[e])
    return out.reshape(B, T, D)


def channel_mixer(h, l, ffn_w1, ffn_w3, ffn_w2, router_w, moe_w1, moe_w3, moe_w2):
    j = l // 2
    if l % 2 == 0:
        return swiglu(h, ffn_w1[j], ffn_w3[j], ffn_w2[j])
    return moe_swiglu(h, router_w[j], moe_w1[j], moe_w3[j], moe_w2[j])


def setup_inputs(seed: int = 0) -> dict:
    key = jax.random.key(seed)
    ks = jax.random.split(key, 32)
    f32 = jnp.float32

    def nrm(k, shape, scale):
        return jax.random.normal(k, shape, f32) * scale

    D = D_MODEL
    i_b = nrm(ks[19], (DEPTH, 2, M_HEADS), 0.1)
    f_b = jnp.linspace(3.0, 6.0, M_HEADS, dtype=f32)[None, None, :] + nrm(ks[20], (DEPTH, 2, M_HEADS), 0.1)
    return {
        'x': nrm(ks[0], (BATCH, SEQ, D), 1.0),
        'c': nrm(ks[1], (BATCH, D), 1.0),
        'ctx': nrm(ks[2], (BATCH, CTX_LEN, D), 1.0),
        'c_ctx': nrm(ks[3], (D,), 1.0),
        'ada_w': nrm(ks[4], (DEPTH, D, 6 * D), 0.5 * D ** -0.5),
        'ada_b': nrm(ks[5], (DEPTH, 6 * D), 0.02),
        'norm1_g': 1.0 + nrm(ks[6], (DEPTH, D), 0.05),
        'norm2_g': 1.0 + nrm(ks[7], (DEPTH, D), 0.05),
        'w_in': nrm(ks[8], (DEPTH, D, IN_COLS), D ** -0.5),
        'w_out': nrm(ks[9], (DEPTH, D_MIX, D), D_MIX ** -0.5),
        'q_norm_g': 1.0 + nrm(ks[10], (DEPTH, A_DH), 0.05),
        'k_norm_g': 1.0 + nrm(ks[11], (DEPTH, A_DH), 0.05),
        'lambda_q1': nrm(ks[12], (DEPTH, A_DH), 0.1),
        'lambda_k1': nrm(ks[13], (DEPTH, A_DH), 0.1),
        'lambda_q2': nrm(ks[14], (DEPTH, A_DH), 0.1),
        'lambda_k2': nrm(ks[15], (DEPTH, A_DH), 0.1),
        'subln_g': 1.0 + nrm(ks[16], (DEPTH, A_DV), 0.05),
        'conv_w': nrm(ks[17], (DEPTH, CONV_K, 2 * M_WIDTH), CONV_K ** -0.5),
        'conv_b': nrm(ks[18], (DEPTH, 2 * M_WIDTH), 0.02),
        'gate_b': jnp.stack([i_b, f_b], axis=2),
        'mnorm_g': 1.0 + nrm(ks[21], (DEPTH, M_WIDTH), 0.05),
        'ffn_w1': nrm(ks[22], (N_DENSE, D, D_FF), D ** -0.5),
        'ffn_w3': nrm(ks[23], (N_DENSE, D, D_FF), D ** -0.5),
        'ffn_w2': nrm(ks[24], (N_DENSE, D_FF, D), D_FF ** -0.5),
        'router_w': nrm(ks[25], (N_MOE, D, N_EXPERTS), D ** -0.5),
        'moe_w1': nrm(ks[26], (N_MOE, N_EXPERTS, D, D_FF_EXPERT), D ** -0.5),
        'moe_w3': nrm(ks[27], (N_MOE, N_EXPERTS, D, D_FF_EXPERT), D ** -0.5),
        'moe_w2': nrm(ks[28], (N_MOE, N_EXPERTS, D_FF_EXPERT, D), D_FF_EXPERT ** -0.5),
    }


def reference(x, c, ctx, c_ctx, ada_w, ada_b, norm1_g, norm2_g, w_in, w_out, q_norm_g, k_norm_g,
              lambda_q1, lambda_k1, lambda_q2, lambda_k2, subln_g, conv_w, conv_b, gate_b, mnorm_g,
              ffn_w1, ffn_w3, ffn_w2, router_w, moe_w1, moe_w3, moe_w2):
    cos, sin = axial_rope_tables(x.shape[1])
    for l in range(DEPTH):
        last = l == DEPTH - 1
        lam_init = 0.8 - 0.6 * jnp.exp(jnp.float32(-0.3 * l))
        lam = (jnp.exp(jnp.sum(lambda_q1[l] * lambda_k1[l]).astype(jnp.float32))
               - jnp.exp(jnp.sum(lambda_q2[l] * lambda_k2[l]).astype(jnp.float32)) + lam_init)
        sh1, sc1, g1, sh2, sc2, g2 = adaln(c, ada_w[l], ada_b[l])
        csh1, csc1, cg1, csh2, csc2, cg2 = adaln(c_ctx, ada_w[l], ada_b[l])
        mix_lat, mix_ctx = hybrid_mixer(
            modulate(x, norm1_g[l], sh1, sc1), modulate(ctx, norm1_g[l], csh1, csc1), cos, sin,
            w_in[l], w_out[l], q_norm_g[l], k_norm_g[l], lam, lam_init, subln_g[l],
            conv_w[l], conv_b[l], gate_b[l], mnorm_g[l], not last)
        x = x + g1 * mix_lat
        x = x + g2 * channel_mixer(modulate(x, norm2_g[l], sh2, sc2), l,
                                   ffn_w1, ffn_w3, ffn_w2, router_w, moe_w1, moe_w3, moe_w2)
        if not last:
            ctx = ctx + cg1 * mix_ctx
            ctx = ctx + cg2 * channel_mixer(modulate(ctx, norm2_g[l], csh2, csc2), l,
                                            ffn_w1, ffn_w3, ffn_w2, router_w, moe_w1, moe_w3, moe_w2)
    return x
```

```python
import numpy as np
from contextlib import ExitStack
import concourse.bass as bass
import concourse.mybir as mybir
from concourse.bass_utils import run_bass_kernel_spmd

F32 = mybir.dt.float32
BF16 = mybir.dt.bfloat16
AF = mybir.ActivationFunctionType
ALU = mybir.AluOpType
AX = mybir.AxisListType

D = 1024
SEQ = 2048
CTX = 256
NT = SEQ + CTX
DEPTH = 2
A_DH = 64
IN_COLS = 3600
D_FF = 2816
D_FFE = 3584
NEXP = 8
EPS = 1e-6
NPAR = 391
SPARSE_MOE = True
SEL_ENG = "dve"
TILES = [(0, 512), (512, 512), (1024, 512), (1536, 512), (2048, 256)]
NBLK = NT // 128


class Buf:
    __slots__ = ("name", "lw", "rd", "rd_dma", "sem", "semcnt")

    def __init__(self, name):
        self.name = name
        self.lw = None
        self.rd = {}
        self.rd_dma = []
        self.sem = None
        self.semcnt = 0


class V:
    __slots__ = ("ap", "bs")

    def __init__(self, ap, bs):
        self.ap = ap
        self.bs = bs


class DV(V):
    __slots__ = ()


class LazyDram:
    def __init__(self, bld, name, shape, dt):
        self.bld, self.name, self.shape, self.dt = bld, name, shape, dt
        self.t = None

    def __getitem__(self, key):
        if self.t is None:
            self.t = self.bld.nc.dram_tensor(self.name, list(self.shape), self.dt, kind="ExternalInput").ap()
            self.bld.dram[self.name] = self.t
        return self.t[key]


class Tn:
    def __init__(self, t, name):
        self.t = t
        self.name = name
        self.bufs = {}

    def v(self, key, *sl):
        ks = key if isinstance(key, list) else [key]
        bs = []
        for k in ks:
            b = self.bufs.get(k)
            if b is None:
                b = self.bufs[k] = Buf(f"{self.name}/{k}")
            bs.append(b)
        ap = self.t[sl] if sl else self.t[:]
        return V(ap, bs)


class Op:
    __slots__ = ("eng", "fn", "deps", "ddeps", "dma", "sem", "val", "waits", "sig", "need")

    def __init__(self, eng, fn):
        self.eng = eng
        self.fn = fn
        self.deps = {}
        self.ddeps = set()
        self.dma = 0
        self.sem = None
        self.val = 0
        self.waits = []
        self.sig = None
        self.need = False


class Prog:
    MAXV = 20000

    def __init__(self, nc, es):
        self.nc = nc
        self.es = es
        self.ops = []
        self.engs = {"pe": nc.tensor, "act": nc.scalar, "dve": nc.vector, "pool": nc.gpsimd, "sp": nc.sync}
        self.last_c = {}
        self.dma_all = []
        self.nsem = 0

    def new_sem(self):
        self.nsem += 1
        return self.es.enter_context(self.nc.semaphore(f"s{self.nsem}"))

    def add(self, eng, fn, reads=(), writes=(), dma=0, sembuf=None):
        idx = len(self.ops)
        op = Op(eng, fn)
        rb = [b for v in reads for b in v.bs]
        wb = [b for v in writes for b in v.bs]

        def dep(j):
            o = self.ops[j]
            if o.dma:
                op.ddeps.add(j)
            else:
                if op.deps.get(o.eng, -1) < j:
                    op.deps[o.eng] = j

        for b in rb:
            if b.lw is not None:
                dep(b.lw)
        for b in wb:
            if b.lw is not None:
                dep(b.lw)
            for j in b.rd.values():
                dep(j)
            for j in b.rd_dma:
                dep(j)
        for b in rb:
            if dma:
                b.rd_dma.append(idx)
            else:
                b.rd[eng] = idx
        for b in wb:
            b.lw = idx
            b.rd = {}
            b.rd_dma = []
        if dma:
            op.dma = dma
            if sembuf.sem is None:
                sembuf.sem = self.new_sem()
            sembuf.semcnt += 16 * dma
            op.sem = sembuf.sem
            op.val = sembuf.semcnt
            self.dma_all.append(idx)
        self.ops.append(op)
        if not dma and fn is not None:
            self.last_c[eng] = idx
        return idx

    def barrier(self):
        lasts = dict(self.last_c)
        dmas = list(self.dma_all)
        self.dma_all = []
        for e in ("pe", "act", "dve", "pool", "sp"):
            op = Op(e, None)
            op.deps = dict(lasts)
            op.ddeps = set(dmas)
            self.ops.append(op)

    def emit(self):
        ops = self.ops
        engs = ("pe", "act", "dve", "pool", "sp")
        waited = {e: {f: -1 for f in engs} for e in engs}
        waited_d = {e: {} for e in engs}
        for i, op in enumerate(ops):
            e = op.eng
            for f, j in op.deps.items():
                if f == "pe" and e == "pe":
                    continue
                if waited[e][f] >= j:
                    continue
                waited[e][f] = j
                ops[j].need = True
                op.waits.append(("c", j))
            for j in op.ddeps:
                o = ops[j]
                if waited_d[e].get(o.sem, 0) >= o.val:
                    continue
                waited_d[e][o.sem] = o.val
                op.waits.append(("d", j))
        cnt = {e: 0 for e in engs}
        sems = {e: [] for e in engs}
        for op in ops:
            if op.need:
                cnt[op.eng] += 1
                n = cnt[op.eng]
                k = (n - 1) // self.MAXV
                while len(sems[op.eng]) <= k:
                    sems[op.eng].append(self.new_sem())
                op.sig = (sems[op.eng][k], n - k * self.MAXV)
        for op in ops:
            eng = self.engs[op.eng]
            for kind, j in op.waits:
                o = ops[j]
                if kind == "c":
                    eng.wait_ge(o.sig[0], o.sig[1])
                else:
                    eng.wait_ge(o.sem, o.val)
            if op.fn is None:
                continue
            ins = op.fn(eng)
            if op.dma:
                if not isinstance(ins, (list, tuple)):
                    ins = [ins]
                assert len(ins) == op.dma
                for x in ins:
                    x.then_inc(op.sem, 16)
            elif op.need:
                ins.then_inc(op.sig[0], 1)
        return cnt


class Builder:
    def __init__(self, stage=99, dbg=False):
        self.stage = stage
        self.dbg = dbg
        self.nc = nc = bass.Bass("TRN2", target_bir_lowering=False)
        self.es = ExitStack()
        self.P = Prog(nc, self.es)
        self.dram = {}
        self.uid = 0
        self.ccols = {}
        self.cct = self.sb("cct", [128, 16], F32)

    def din(self, name, shape, dt=F32):
        return LazyDram(self, name, shape, dt)

    def dout(self, name, shape, dt=F32):
        t = self.nc.dram_tensor(name, list(shape), dt, kind="ExternalOutput").ap()
        self.dram[name] = t
        return t

    def sb(self, name, shape, dt, es=None):
        self.uid += 1
        t = (es or self.es).enter_context(self.nc.sbuf_tensor(f"{name}_{self.uid}", list(shape), dt))
        return Tn(t, name)

    def mm(self, out, lhsT, rhs, start=True, stop=True, skip=False):
        if skip:
            self.P.add("pe", lambda e: e.matmul(out.ap, lhsT.ap, rhs.ap, start=start, stop=stop, skip_group_check=True),
                       reads=[lhsT, rhs], writes=[out])
        else:
            self.P.add("pe", lambda e: e.matmul(out.ap, lhsT.ap, rhs.ap, start=start, stop=stop),
                       reads=[lhsT, rhs], writes=[out])

    def tr(self, out, in_, ident):
        self.P.add("pe", lambda e: e.transpose(out.ap, in_.ap, ident.ap), reads=[in_, ident], writes=[out])

    def act(self, out, in_, func, bias=None, scale=None, accum=None):
        kw = {}
        rd = [in_]
        wr = [out]
        if bias is not None:
            if isinstance(bias, V):
                kw["bias"] = bias.ap
                rd.append(bias)
            else:
                kw["bias"] = bias
        if scale is not None:
            if isinstance(scale, V):
                kw["scale"] = scale.ap
                rd.append(scale)
            else:
                kw["scale"] = scale
        if accum is not None:
            kw["accum_out"] = accum.ap
            wr.append(accum)
        self.P.add("act", lambda e: e.activation(out.ap, in_.ap, func, **kw), reads=rd, writes=wr)

    def tt(self, out, in0, in1, op, eng="dve"):
        self.P.add(eng, lambda e: e.tensor_tensor(out.ap, in0.ap, in1.ap, op), reads=[in0, in1], writes=[out])

    def ts(self, out, in0, s1, op0, s2=None, op1=None, eng="dve", accum=None):
        rd = [in0]
        wr = [out]
        a1 = s1
        a2 = s2
        if isinstance(s1, V):
            rd.append(s1)
            a1 = s1.ap
        if isinstance(s2, V):
            rd.append(s2)
            a2 = s2.ap
        kw = {}
        if op1 is not None:
            kw["op1"] = op1
        if accum is not None:
            kw["accum_out"] = accum.ap
            wr.append(accum)
        self.P.add(eng, lambda e: e.tensor_scalar(out.ap, in0.ap, a1, a2, op0, **kw), reads=rd, writes=wr)

    def stt(self, out, in0, scalar, in1, op0, op1, eng="dve"):
        rd = [in0, in1]
        a = scalar
        if isinstance(scalar, V):
            rd.append(scalar)
            a = scalar.ap
        self.P.add(eng, lambda e: e.scalar_tensor_tensor(out.ap, in0.ap, a, in1.ap, op0, op1), reads=rd, writes=[out])

    def copy(self, out, in_, eng="dve"):
        if eng == "act":
            self.P.add("act", lambda e: e.copy(out.ap, in_.ap), reads=[in_], writes=[out])
        else:
            self.P.add(eng, lambda e: e.tensor_copy(out.ap, in_.ap), reads=[in_], writes=[out])

    def rsqrt(self, out, in_, addc):
        self.act(out, in_, AF.Ln, bias=self.constcol(addc))
        self.act(out, out, AF.Exp, scale=-0.5)

    def constcol(self, val):
        key = float(val)
        if key not in self.ccols:
            i = len(self.ccols)
            v = self.cct.v(i, slice(None), slice(i, i + 1))
            self.memset(v, key)
            self.ccols[key] = v
        return self.ccols[key]

    def recip(self, out, in_, dve=False):
        n = 1
        for d in out.ap.shape[1:]:
            n *= d
        if n <= 16 or dve:
            self.P.add("dve", lambda e: e.reciprocal(out.ap, in_.ap), reads=[in_], writes=[out])
        else:
            self.act(out, in_, AF.Ln)
            self.act(out, out, AF.Exp, scale=-1.0)

    def rmax(self, out, in_):
        self.P.add("dve", lambda e: e.reduce_max(out.ap, in_.ap, AX.X), reads=[in_], writes=[out])

    def memset(self, out, val, eng="dve"):
        self.P.add(eng, lambda e: e.memset(out.ap, val), writes=[out])

    def dma(self, q, out, in_, pairs=None):
        if pairs is None:
            pairs = [(out.ap, in_.ap)]
        n = len(pairs)

        def fn(e):
            return [e.dma_start(out=o, in_=i) for o, i in pairs]

        sembuf = in_.bs[0] if isinstance(out, DV) else out.bs[0]
        self.P.add(q, fn, reads=[in_], writes=[out], dma=n, sembuf=sembuf)


    def build(self):
        nc, P = self.nc, self.P
        es = self.es
        x_d = self.din("x", [SEQ, D])
        ctx_d = self.din("ctx", [CTX, D])
        cvec_d = self.din("cvec", [128, 16])
        pp_d = self.din("pp", [128, DEPTH * NPAR])
        cbf_d = self.din("cbf", [128, 6 * 128])
        cf32_d = self.din("cf32", [128, 4 * 128])
        sel_d = self.din("sel", [8, 8 * 128])
        rope_d = self.din("rope", [128, 2 * SEQ])
        ada_w_d = self.din("ada_w", [DEPTH, D, 6 * D])
        w_in_d = self.din("w_in", [DEPTH, D, IN_COLS])
        w_out_d = self.din("w_out", [DEPTH, D, D])
        ffn_w1_d = self.din("ffn_w1", [1, D, D_FF])
        ffn_w3_d = self.din("ffn_w3", [1, D, D_FF])
        ffn_w2_d = self.din("ffn_w2", [1, D_FF, D])
        router_d = self.din("router_w", [1, D, NEXP])
        moe_w1_d = self.din("moe_w1", [1, NEXP, D, D_FFE])
        moe_w3_d = self.din("moe_w3", [1, NEXP, D, D_FFE])
        moe_w2_d = self.din("moe_w2", [1, NEXP, D_FFE, D])
        y_d = self.dout("y", [SEQ, D])
        self.dbg_outs = {}

        self.ps = [Tn(es.enter_context(nc.psum_tensor(f"ps{i}", [128, 512], F32)), f"ps{i}") for i in range(8)]
        ps = self.ps

        xT = self.sb("xT", [128, 8, NT], F32)
        hT = None
        cbf = self.sb("cbf", [128, 6 * 128], BF16)
        cf32 = self.sb("cf32", [128, 4 * 128], F32)
        sel = self.sb("sel", [8, 8 * 128], BF16) if not SPARSE_MOE else None
        pp = self.sb("pp", [128, DEPTH * NPAR], F32)
        cvec = self.sb("cvec", [128, 16], F32)
        self.xT, self.hT = xT, hT

        q = "pool"
        self.dma("sp", pp.v(0), DV(pp_d[:, :], [Buf("d")]))
        self.dma("sp", cvec.v(0), DV(cvec_d[:, :], [Buf("d")]))
        self.dma("sp", cf32.v(0), DV(cf32_d[:, :], [Buf("d")]))
        self.dma(q, cbf.v(0), DV(cbf_d[:, :], [Buf("d")]))
        if sel is not None:
            self.dma(q, sel.v(0), DV(sel_d[:, :], [Buf("d")]))

        def cb(i):
            return cbf.v(0, slice(None), slice(i * 128, (i + 1) * 128))

        def cf(i):
            return cf32.v(0, slice(None), slice(i * 128, (i + 1) * 128))

        self.ident_bf, self.ones_bf, self.blk64, self.perm, self.maskF, self.maskB = [cb(i) for i in range(6)]
        self.ident_f, self.triF, self.triB, self.ones_f = [cf(i) for i in range(4)]
        self.sel = sel
        self.pp = pp
        self.cvec = cvec

        S = slice
        ALL = slice(None)

        def tile_of_blk(b):
            return b // 4 if b < 16 else 4

        self.tile_of_blk = tile_of_blk

        self.lay = {}
        self.lay_t = {}
        for l_ in range(DEPTH):
            self.lay_t[l_] = (self.sb("mod", [128, 48, 2], F32),
                              self.sb("AB", [128, 2, 8, 2], F32),
                              self.sb("scb", [128, 8, 2], BF16),
                              self.sb("smal", [128, 16], F32))
        with ExitStack() as es0:
            stg = self.sb("xstage", [128, 2, D], F32, es0)
            adaw = self.sb("adaw", [128, 3, 8, 512], BF16, es0)
            junk = self.sb("junk", [128, 2, 64], F32, es0)
            self.prologue(0, ada_w_d, adaw, junk)
            for tb in range(NBLK):
                src = x_d[tb * 128:(tb + 1) * 128, :] if tb < 16 else ctx_d[(tb - 16) * 128:(tb - 15) * 128, :]
                self.dma("sp", stg.v(tb % 2, ALL, tb % 2, ALL), DV(src, [Buf("d")]))
                ti = tile_of_blk(tb)
                for half in range(2):
                    bank = ps[(tb * 2 + half) % 4]
                    for cc in range(4):
                        c = half * 4 + cc
                        self.tr(bank.v(0, ALL, S(cc * 128, (cc + 1) * 128)),
                                stg.v(tb % 2, ALL, tb % 2, S(c * 128, (c + 1) * 128)), self.ident_f)
                    dst = xT.v([(c, ti) for c in range(half * 4, half * 4 + 4)], ALL, S(half * 4, half * 4 + 4),
                               S(tb * 128, (tb + 1) * 128))
                    srcv = V(bank.t[:, :].rearrange("p (a b) -> p a b", a=4), bank.v(0).bs)
                    self.copy(dst, srcv, eng="act" if half == 0 else "dve")
            self.prologue(1, ada_w_d, adaw, junk)
        if self.stage <= -1:
            return self.finish_debug({"xT": (xT, F32, [128, 8, NT])})
        P.barrier()

        for l in range(DEPTH):
            r = self.layer(l, ada_w_d, w_in_d, w_out_d, ffn_w1_d, ffn_w3_d, ffn_w2_d, router_d,
                           moe_w1_d, moe_w3_d, moe_w2_d, rope_d)
            if r is not None:
                return r

        with ExitStack() as eso:
            ost = self.sb("ostage", [128, 2, D], F32, eso)
            outs = []
            for tb in range(16):
                ti = tile_of_blk(tb)
                for half in range(2):
                    bank = ps[(tb * 2 + half) % 4]
                    for cc in range(4):
                        c = half * 4 + cc
                        self.tr(bank.v(0, ALL, S(cc * 128, (cc + 1) * 128)),
                                xT.v((c, ti), ALL, c, S(tb * 128, (tb + 1) * 128)), self.ident_f)
                    self.copy(ost.v((tb % 2, half), ALL, tb % 2, S(half * 512, (half + 1) * 512)), bank.v(0),
                              eng="act" if half == 0 else "dve")
                ov = DV(y_d[tb * 128:(tb + 1) * 128, :], [Buf("yout")])
                self.dma("sp", ov, ost.v([(tb % 2, 0), (tb % 2, 1)], ALL, tb % 2, ALL))
                outs.append(ov)
            P.add("sp", None, reads=outs)
        self.P.emit()
        return None

    def finish_debug(self, tensors):
        self.P.barrier()
        outs = []
        for name, (tn, dt, shape) in tensors.items():
            d = self.dout("dbg_" + name, shape, dt)
            ov = DV(d[tuple(slice(None) for _ in shape)], [Buf("dbg")])
            allb = list(tn.bufs.keys())
            self.dma("sp", ov, tn.v(allb))
            outs.append(ov)
        self.P.add("sp", None, reads=outs)
        self.P.emit()
        return list(tensors.keys())


def _consts():
    p = np.arange(128)
    ident = np.eye(128, dtype=np.float32)
    ones = np.ones((128, 128), np.float32)
    blk64 = (p[:, None] // 64 == p[None, :] // 64).astype(np.float32)
    perm = np.zeros((128, 128), np.float32)
    for j in range(128):
        if j % 64 < 32:
            perm[j + 32, j] = -1.0
        else:
            perm[j - 32, j] = 1.0
    maskF = (p[:, None] <= p[None, :]).astype(np.float32)
    maskB = (p[:, None] >= p[None, :]).astype(np.float32)
    cbf = np.concatenate([ident, ones, blk64, perm, maskF, maskB], axis=1)
    cf32 = np.concatenate([ident, maskF, maskB, ones], axis=1)
    sel = np.zeros((8, 8, 128), np.float32)
    for e in range(8):
        sel[e, e, :] = 1.0
    sel = sel.reshape(8, 8 * 128)
    rows = SEQ // 64
    row = np.repeat(np.arange(rows, dtype=np.float32), 64)
    col = np.tile(np.arange(64, dtype=np.float32), rows)
    inv = (10000.0 ** (-np.arange(16, dtype=np.float32) / 16)).astype(np.float32)
    ang = np.concatenate([row[:, None] * inv, col[:, None] * inv], axis=-1)
    cosT = np.cos(ang).T.astype(np.float32)
    sinT = np.sin(ang).T.astype(np.float32)
    C = np.tile(cosT, (4, 1))
    Sn = np.tile(sinT, (4, 1))
    rope = np.concatenate([C, Sn], axis=1).astype(np.float32)
    iota = np.broadcast_to(np.arange(512, dtype=np.float32)[None, :], (128, 512)).copy()
    return cbf, cf32, sel, rope, iota


def _pack_params(inp):
    pp = np.zeros((DEPTH, 128, NPAR), np.float32)

    def pc(v):
        return np.ascontiguousarray(v.reshape(-1, 128).T)

    for l in range(DEPTH):
        pp[l, :, 0:8] = pc(inp["norm1_g"][l])
        pp[l, :, 8:16] = pc(inp["norm2_g"][l])
        pp[l, :, 16:64] = pc(inp["ada_b"][l])
        pp[l, :, 64] = np.tile(inp["q_norm_g"][l], 2)
        pp[l, :, 65] = np.tile(inp["k_norm_g"][l], 2)
        pp[l, :, 66] = inp["subln_g"][l]
        pp[l, :, 67:71] = pc(inp["mnorm_g"][l])
        cw = inp["conv_w"][l]
        for j in range(5):
            pp[l, :, 71 + j * 8:71 + (j + 1) * 8] = pc(cw[j])
        pp[l, :, 111:119] = pc(inp["conv_b"][l])
        pp[l, :, 119:135] = np.broadcast_to(inp["gate_b"][l].reshape(1, 16), (128, 16))
        pp[l, :, 135:199] = np.broadcast_to(inp["lambda_q1"][l][None, :], (128, 64))
        pp[l, :, 199:263] = np.broadcast_to(inp["lambda_k1"][l][None, :], (128, 64))
        pp[l, :, 263:327] = np.broadcast_to(inp["lambda_q2"][l][None, :], (128, 64))
        pp[l, :, 327:391] = np.broadcast_to(inp["lambda_k2"][l][None, :], (128, 64))
    return np.ascontiguousarray(pp.transpose(1, 0, 2).reshape(128, DEPTH * NPAR))


def _in_maps(inp):
    inp = {k: np.asarray(v, dtype=np.float32) for k, v in inp.items()}
    cbf, cf32, sel, rope, iota = _consts()
    pp = _pack_params(inp)
    shared = {
        "pp": pp, "cbf": cbf, "cf32": cf32, "sel": sel, "rope": rope, "iota": iota,
        "ada_w": inp["ada_w"], "w_in": inp["w_in"], "w_out": inp["w_out"],
        "ffn_w1": inp["ffn_w1"], "ffn_w3": inp["ffn_w3"], "ffn_w2": inp["ffn_w2"],
        "router_w": inp["router_w"], "moe_w1": inp["moe_w1"], "moe_w3": inp["moe_w3"], "moe_w2": inp["moe_w2"],
    }
    if SPARSE_MOE:
        w1, w3, w2 = inp["moe_w1"][0], inp["moe_w3"][0], inp["moe_w2"][0]
        for b in range(14):
            shared[f"m1_{b}"] = np.ascontiguousarray(w1[:, :, b * 256:(b + 1) * 256])
            shared[f"m3_{b}"] = np.ascontiguousarray(w3[:, :, b * 256:(b + 1) * 256])
        for rp in range(7):
            for dh in range(2):
                shared[f"m2_{rp}_{dh}"] = np.ascontiguousarray(w2[:, rp * 512:(rp + 1) * 512, dh * 512:(dh + 1) * 512])
    maps = []
    cc = inp["c_ctx"].reshape(8, 128).T
    for b in range(8):
        m = dict(shared)
        m["x"] = np.ascontiguousarray(inp["x"][b])
        m["ctx"] = np.ascontiguousarray(inp["ctx"][b])
        cv = np.zeros((128, 16), np.float32)
        cv[:, 0:8] = inp["c"][b].reshape(8, 128).T
        cv[:, 8:16] = cc
        m["cvec"] = cv
        maps.append(m)
    return maps


def kernel(**inputs):
    bld = Builder()
    bld.build()
    maps = _in_maps(inputs)
    used = set(bld.dram.keys())
    maps = [{k: v for k, v in m.items() if k in used} for m in maps]
    res = run_bass_kernel_spmd(bld.nc, maps, core_ids=list(range(8)))
    bld.es.close()
    return np.stack([np.asarray(r["y"], dtype=np.float32) for r in res.results], axis=0)


S_ = slice
ALL_ = slice(None)


def _prologue(self, l, ada_w_d, wsl, junk):
    P, ps, pp = self.P, self.ps, self.pp
    S, ALL = S_, ALL_
    lam_init = float(0.8 - 0.6 * np.exp(np.float32(-0.3 * l)))
    pb = l * NPAR

    def ppv(a, b=None):
        if b is None:
            return pp.v(0, ALL, S(pb + a, pb + a + 1))
        return pp.v(0, ALL, S(pb + a, pb + b))

    mod, AB, scb, smal = self.lay_t[l]
    for s in range(2):
        self.act(scb.v(0, ALL, ALL, s), self.cvec_v(s), AF.Silu)
    pm = ps[7]
    for blk in range(12):
        sl = blk % 3
        src = ada_w_d[l, :, blk * 512:(blk + 1) * 512].rearrange("(k p) c -> p k c", p=128)
        self.dma("pool", wsl.v(sl, ALL, sl, ALL, ALL), DV(src, [Buf("d")]))
        for j in range(4):
            col = (blk * 4 + j) * 2
            for k in range(8):
                self.mm(pm.v(0, ALL, S(col, col + 2)), wsl.v(sl, ALL, sl, k, S(j * 128, (j + 1) * 128)),
                        scb.v(0, ALL, k, ALL), start=(k == 0), stop=(k == 7))
    pmv = V(pm.t[:, 0:96].rearrange("p (a b) -> p a b", b=2), pm.v(0).bs)
    for s in range(2):
        self.tt(mod.v(0, ALL, ALL, s), V(pmv.ap[:, :, s], pmv.bs), ppv(16, 64), ALU.add)
    for which in range(2):
        sc_i = 1 if which == 0 else 4
        for s in range(2):
            self.stt(AB.v(0, ALL, which, ALL, s), mod.v(0, ALL, S(sc_i * 8, sc_i * 8 + 8), s), 1.0,
                     ppv(which * 8, which * 8 + 8), ALU.add, ALU.mult)
    self.ts(AB.v(0), AB.v(0), float(np.sqrt(D)), ALU.mult)
    self.tt(junk.v(0, ALL, 0, ALL), ppv(135, 199), ppv(199, 263), ALU.mult)
    self.P.add("dve", lambda e: e.reduce_sum(smal.t[:, 0:1], junk.t[:, 0, :], AX.X),
               reads=[junk.v(0)], writes=[smal.v(0)])
    self.tt(junk.v(1, ALL, 1, ALL), ppv(263, 327), ppv(327, 391), ALU.mult)
    self.P.add("dve", lambda e: e.reduce_sum(smal.t[:, 1:2], junk.t[:, 1, :], AX.X),
               reads=[junk.v(1)], writes=[smal.v(1)])
    self.act(smal.v(2, ALL, S(2, 4)), smal.v([0, 1], ALL, S(0, 2)), AF.Exp)
    self.stt(smal.v(4, ALL, S(4, 5)), smal.v(2, ALL, S(3, 4)), -lam_init, smal.v(2, ALL, S(2, 3)),
             ALU.add, ALU.subtract)
    self.ts(smal.v(5, ALL, S(5, 7)), ppv(64, 66), 8.0, ALU.mult)
    self.ts(smal.v(7, ALL, S(7, 8)), ppv(66), float((1.0 - lam_init) * np.sqrt(128.0)), ALU.mult)
    self.ts(smal.v(8, ALL, S(8, 12)), ppv(67, 71), float(np.sqrt(128.0)), ALU.mult)
    self.lay[l] = (mod, AB, smal)


def _layer(self, l, ada_w_d, w_in_d, w_out_d, ffn_w1_d, ffn_w3_d, ffn_w2_d, router_d,
           moe_w1_d, moe_w3_d, moe_w2_d, rope_d):
    P, ps, xT, pp = self.P, self.ps, self.xT, self.pp
    S, ALL = S_, ALL_
    last = l == DEPTH - 1
    lam_init = float(0.8 - 0.6 * np.exp(np.float32(-0.3 * l)))
    pb = l * NPAR

    def ppv(a, b=None):
        if b is None:
            return pp.v(0, ALL, S(pb + a, pb + a + 1))
        return pp.v(0, ALL, S(pb + a, pb + b))

    tiles = TILES if not last else TILES
    out_tiles = TILES if not last else TILES[:4]

    mod, AB, smal = self.lay[l]

    def modv(i, c, s):
        return mod.v(0, ALL, S(i * 8 + c, i * 8 + c + 1), s)

    def ABv(which, c, s):
        return AB.v(0, ALL, which, S(c, c + 1), s)

    if True:
        with ExitStack() as esm:
            hT = self.hT = self.sb("hT", [128, 8, NT], BF16, esm)
            self.norm_mod(0, TILES, ABv, modv, 0)
            if self.stage == l * 10 + 1:
                return self.finish_debug({"hT": (hT, BF16, [128, 8, NT])})
            P.barrier()
            self.slots = self.sb("wslot", [128, 3, 4096], BF16, esm)
            self.nslots = 3
            self.slot_i = 0
            catA = self.sb("catA", [128, 4, NT], BF16, esm)
            self.attn_heads(l, w_in_d, rope_d, catA, smal, last)
            if self.stage == l * 10 + 2:
                return self.finish_debug({"catA": (catA, BF16, [128, 4, NT])})
            out_tiles = [0, 1, 2, 3] if last else [0, 1, 2, 3, 4]
            self.w_out_half(l, w_out_d, 0, catA, out_tiles, modv)
            P.barrier()
            gsc = self.gates(l, w_in_d, esm)
            if self.stage == l * 10 + 3:
                return self.finish_debug({"ga": (gsc[0], F32, [128, NBLK, 8]), "gc": (gsc[1], F32, [128, NBLK, 8]),
                                          "gib": (gsc[2], F32, [128, NBLK, 8]), "gd": (gsc[3], F32, [128, NBLK, 8])})
            self.mlstm_heads(l, w_in_d, catA, smal, last, gsc)
            if self.stage == l * 10 + 4:
                return self.finish_debug({"catM": (catA, BF16, [128, 4, NT])})
            self.w_out_half(l, w_out_d, 1, catA, out_tiles, modv)
            if self.stage == l * 10 + 5:
                return self.finish_debug({"xT": (xT, F32, [128, 8, NT])})
            P.barrier()
        if l % 2 == 0:
            with ExitStack() as esh:
                hT = self.hT = self.sb("hT", [128, 8, NT], BF16, esh)
                self.norm_mod(1, TILES, ABv, modv, 3)
                if self.stage == l * 10 + 6:
                    return self.finish_debug({"hT": (hT, BF16, [128, 8, NT])})
                r = self.ffn_dense(l, ffn_w1_d, ffn_w3_d, ffn_w2_d, modv)
                if r is not None:
                    return r
        else:
            if SPARSE_MOE:
                r = self.moe_sparse(l, router_d, ABv, modv)
            else:
                with ExitStack() as esh:
                    self.hT = self.sb("hT", [128, 8, NT], BF16, esh)
                    r = self.moe(l, router_d, moe_w1_d, moe_w3_d, moe_w2_d, ABv, modv)
            if r is not None:
                return r
        if self.stage == l * 10 + 7:
            return self.finish_debug({"xT": (xT, F32, [128, 8, NT])})
        P.barrier()
    return None


def _cvec_v(self, s):
    return self.cvec.v(0, ALL_, S_(s * 8, s * 8 + 8))


def _norm_mod(self, which, tiles, ABv, modv, shift_i, router=None, dst=None, after_tile=None):
    ps, xT, hT = self.ps, self.xT, self.hT
    S, ALL = S_, ALL_
    with ExitStack() as esn:
        sq = self.sb("nsq", [128, 2, 512], BF16, esn)
        rstd = self.sb("nrstd", [128, 2, 512], F32, esn)
        tmp = self.sb("ntmp", [128, 2, 512], F32, esn)
        hf = self.sb("nhf", [128, 2, 512], F32, esn) if router is not None else None
        for ti, (t0, n) in enumerate(TILES):
            if (t0, n) not in tiles:
                continue
            s = 0 if ti < 4 else 1
            bank = ps[ti % 2]
            for c in range(8):
                self.act(sq.v(c % 2, ALL, c % 2, S(0, n)), xT.v((c, ti), ALL, c, S(t0, t0 + n)), AF.Square)
                self.mm(bank.v(0, ALL, S(0, n)), self.ones_bf, sq.v(c % 2, ALL, c % 2, S(0, n)),
                        start=(c == 0), stop=(c == 7))
            r = rstd.v(ti % 2, ALL, ti % 2, S(0, n))
            self.rsqrt(r, bank.v(0, ALL, S(0, n)), float(D * EPS))
            for c in range(8):
                t = tmp.v(c % 2, ALL, c % 2, S(0, n))
                self.tt(t, xT.v((c, ti), ALL, c, S(t0, t0 + n)), r, ALU.mult)
                if router is None:
                    self.act(hT.v((c, ti), ALL, c, S(t0, t0 + n)), t, AF.Identity,
                             bias=modv(shift_i, c, s), scale=ABv(which, c, s))
                else:
                    wr, pR = router
                    hv = hf.v(c % 2, ALL, c % 2, S(0, n))
                    self.act(hv, t, AF.Identity, bias=modv(shift_i, c, s), scale=ABv(which, c, s))
                    self.copy(dst(c, ti, n) if dst is not None else hT.v((c, ti), ALL, c, S(t0, t0 + n)), hv)
                    for bi in range(n // 128):
                        col = (ti * 4 + bi) * 8
                        self.mm(pR.v(0, ALL, S(col, col + 8)), hf.v(c % 2, ALL, c % 2, S(bi * 128, (bi + 1) * 128)),
                                wr.v(0, ALL, c, ALL), start=(c == 0 and bi == 0), stop=(c == 7), skip=True)
            if after_tile is not None:
                after_tile(ti)
    self.P.barrier()


Builder.layer = _layer
Builder.prologue = _prologue
Builder.cvec_v = _cvec_v
Builder.norm_mod = _norm_mod


def _slot(self):
    i = self.slot_i % self.nslots
    self.slot_i += 1
    return i


def _attn_heads(self, l, w_in_d, rope_d, catA, smal, last):
    P, ps, hT = self.P, self.ps, self.hT
    S, ALL = S_, ALL_
    slots = self.slots
    tob = self.tile_of_blk
    neglam = smal.v(4, ALL, S(4, 5))
    gs = smal.v(7, ALL, S(7, 8))
    with ExitStack() as esw:
        rope = self.sb("rope", [128, 2 * SEQ], BF16, esw)
        qT = self.sb("qT", [128, NT], BF16, esw)
        kT = self.sb("kT", [128, NT], BF16, esw)
        vtok = self.sb("vtok", [128, NBLK, 128], BF16, esw)
        pT = self.sb("pT", [128, 4, 512], BF16, esw)
        sqb = self.sb("sqb", [128, 2, 512], BF16, esw)
        qn = self.sb("qn", [128, 2, 512], BF16, esw)
        tf = self.sb("tf", [128, 6, 512], F32, esw)
        self.dma("pool", rope.v(0), DV(rope_d[:, :], [Buf("d")]))
        for h in range(4):
            si = self.slot()
            wv = slots.t[:, si, 0:8 * 384].rearrange("p (k c) -> p k c", c=384)
            wb = slots.v(si).bs
            pairs = []
            for j, base in enumerate((0, 512, 1024)):
                col = base + h * 128
                pairs.append((wv[:, :, j * 128:(j + 1) * 128],
                              w_in_d[l, :, col:col + 128].rearrange("(k p) c -> p k c", p=128)))
            self.dma("pool", V(wv, wb), DV(pairs[0][1], [Buf("d")]), pairs=pairs)
            groups = []
            for which, dest in ((0, qT), (1, kT)):
                for ti, (t0, n) in enumerate(TILES):
                    if which == 0 and last and ti == 4:
                        continue
                    groups.append((which, dest, ti, t0, n))
            pa = [ps[0], ps[1], ps[2]]
            pbk = [ps[3], ps[4]]
            pck = [ps[5], ps[6]]

            def stageA(gi):
                which, dest, ti, t0, n = groups[gi]
                bA = pa[gi % 3]
                for k in range(8):
                    self.mm(bA.v(0, ALL, S(0, n)), V(wv[:, k, which * 128:(which + 1) * 128], wb),
                            hT.v((k, ti), ALL, k, S(t0, t0 + n)), start=(k == 0), stop=(k == 7))

            stageA(0)
            if len(groups) > 1:
                stageA(1)
            for gi, (which, dest, ti, t0, n) in enumerate(groups):
                g8 = smal.v(5 + which, ALL, S(5 + which, 6 + which))
                par = gi % 2
                bA, bB, bC = pa[gi % 3], pbk[par], pck[par]
                sq = sqb.v(par, ALL, par, S(0, n))
                self.act(sq, bA.v(0, ALL, S(0, n)), AF.Square)
                self.mm(bB.v(0, ALL, S(0, n)), self.blk64, sq)
                if gi + 2 < len(groups):
                    stageA(gi + 2)
                rstd = tf.v(par, ALL, par, S(0, n))
                self.rsqrt(rstd, bB.v(0, ALL, S(0, n)), float(64 * EPS))
                dv = dest.v(ti, ALL, S(t0, t0 + n))
                if ti < 4:
                    qv = qn.v(par, ALL, par, S(0, n))
                    self.stt(qv, bA.v(0, ALL, S(0, n)), g8, rstd, ALU.mult, ALU.mult)
                    self.mm(bC.v(0, ALL, S(0, n)), self.perm, qv)
                    t1 = tf.v(2 + par, ALL, 2 + par, S(0, n))
                    t2 = tf.v(4 + par, ALL, 4 + par, S(0, n))
                    self.tt(t1, qv, rope.v(0, ALL, S(t0, t0 + n)), ALU.mult)
                    self.tt(t2, bC.v(0, ALL, S(0, n)), rope.v(0, ALL, S(SEQ + t0, SEQ + t0 + n)), ALU.mult)
                    self.tt(dv, t1, t2, ALU.add)
                else:
                    self.stt(dv, bA.v(0, ALL, S(0, n)), g8, rstd, ALU.mult, ALU.mult)
            for g in range(5):
                blks = list(range(g * 4, min(g * 4 + 4, NBLK)))
                bank = ps[7 - g % 2]
                for bi, blk in enumerate(blks):
                    for k in range(8):
                        self.mm(bank.v(0, ALL, S(bi * 128, (bi + 1) * 128)),
                                hT.v((k, tob(blk)), ALL, k, S(blk * 128, (blk + 1) * 128)),
                                V(wv[:, k, 256:384], wb), start=(k == 0), stop=(k == 7))
                nb = len(blks)
                srcv = V(bank.t[:, 0:nb * 128].rearrange("p (a b) -> p a b", b=128), bank.v(0).bs)
                self.copy(vtok.v(g, ALL, S(blks[0], blks[0] + nb), ALL), srcv, eng="act")
            qtiles = [(ti, list(range(NBLK))) for ti in range(4)]
            if not last:
                qtiles.append((4, [16, 17]))
            pending = []
            for ti, kblks in qtiles:
                t0, n = TILES[ti]
                psS = [[ps[0], ps[1]], [ps[2], ps[3]]]
                psO = [ps[4], ps[5]]
                psZ = [ps[6], ps[7]]
                nk = len(kblks)

                def Sstep(i):
                    kb = kblks[i]
                    for c in range(2):
                        self.mm(psS[c][i % 2].v(0, ALL, S(0, n)),
                                kT.v(tob(kb), S(c * 64, (c + 1) * 64), S(kb * 128, (kb + 1) * 128)),
                                qT.v(ti, S(c * 64, (c + 1) * 64), S(t0, t0 + n)))

                Sstep(0)
                for i in range(nk):
                    if i + 1 < nk:
                        Sstep(i + 1)
                    kb = kblks[i]
                    for c in range(2):
                        pv = pT.v((c, i % 2), ALL, c * 2 + i % 2, S(0, n))
                        self.act(pv, psS[c][i % 2].v(0, ALL, S(0, n)), AF.Exp, scale=0.125)
                    for c in range(2):
                        pv = pT.v((c, i % 2), ALL, c * 2 + i % 2, S(0, n))
                        self.mm(psO[c].v(0, ALL, S(0, n)), vtok.v(kb // 4, ALL, kb, ALL), pv,
                                start=(i == 0), stop=(i == nk - 1))
                        self.mm(psZ[c].v(0, ALL, S(0, n)), self.ones_bf, pv,
                                start=(i == 0), stop=(i == nk - 1))
                    if pending and i in (1, 9):
                        pending.pop(0)()
                while pending:
                    pending.pop(0)()
                zc1 = tf.v(0, ALL, 0, S(0, n))
                zc2 = tf.v(1, ALL, 1, S(0, n))
                o1 = tf.v(2, ALL, 2, S(0, n))
                o2 = tf.v(3, ALL, 3, S(0, n))
                rs = tf.v(4, ALL, 4, S(0, n))
                self.copy(zc1, psZ[0].v(0, ALL, S(0, n)), eng="dve")
                self.copy(zc2, psZ[1].v(0, ALL, S(0, n)), eng="dve")
                self.copy(o1, psO[0].v(0, ALL, S(0, n)), eng="act")
                self.copy(o2, psO[1].v(0, ALL, S(0, n)), eng="act")

                def fin1(zc1=zc1, zc2=zc2, o1=o1, o2=o2, n=n):
                    self.recip(zc1, zc1)
                    self.recip(zc2, zc2)
                    self.tt(o1, o1, zc1, ALU.mult)
                    self.tt(o2, o2, zc2, ALU.mult)
                    self.stt(o1, o2, neglam, o1, ALU.mult, ALU.add)
                    self.act(sqb.v(0, ALL, 0, S(0, n)), o1, AF.Square)

                def fin2(o1=o1, rs=rs, n=n, h=h, ti=ti, t0=t0):
                    sq = sqb.v(0, ALL, 0, S(0, n))
                    self.mm(ps[1].v(0, ALL, S(0, n)), self.ones_bf, sq)
                    self.rsqrt(rs, ps[1].v(0, ALL, S(0, n)), float(128 * EPS))
                    self.stt(catA.v((h, ti), ALL, h, S(t0, t0 + n)), o1, gs, rs, ALU.mult, ALU.mult)

                pending.extend([fin1, fin2])
            while pending:
                pending.pop(0)()


Builder.slot = _slot
Builder.attn_heads = _attn_heads


def _gates(self, l, w_in_d, esm):
    ps, hT, pp = self.ps, self.hT, self.pp
    S, ALL = S_, ALL_
    tob = self.tile_of_blk
    pb = l * NPAR
    lns = float(-0.5 * np.log(128.0))
    G = self.sb("G", [128, NBLK, 16], F32, esm)
    sp = self.sb("sp", [128, NBLK, 8], F32, esm)
    Bs = self.sb("Bs", [128, NBLK, 8], F32, esm)
    Bt = self.sb("Bt", [128, NBLK, 8], F32, esm)
    E1 = self.sb("E1", [128, NBLK, 8], F32, esm)
    ga = self.sb("ga", [128, NBLK, 8], F32, esm)
    gc = self.sb("gc", [128, NBLK, 8], F32, esm)
    gib = self.sb("gib", [128, NBLK, 8], F32, esm)
    gd = self.sb("gd", [128, NBLK, 8], F32, esm)
    si = self.slot()
    wv = self.slots.t[:, si, 0:128].rearrange("p (k c) -> p k c", c=16)
    wb = self.slots.v(si).bs
    self.dma("pool", V(wv, wb), DV(w_in_d[l, :, 3584:3600].rearrange("(k p) c -> p k c", p=128), [Buf("d")]))
    pG = ps[0]
    for blk in range(NBLK):
        for k in range(8):
            self.mm(pG.v(0, ALL, S(blk * 16, blk * 16 + 16)), hT.v((k, tob(blk)), ALL, k, S(blk * 128, (blk + 1) * 128)),
                    V(wv[:, k, :], wb), start=(k == 0), stop=(k == 7))
    for blk in range(NBLK):
        self.tt(G.v(0, ALL, blk, ALL), pG.v(0, ALL, S(blk * 16, blk * 16 + 16)), pp.v(0, ALL, S(pb + 119, pb + 135)),
                ALU.add)
    for d in range(2):
        self.act(sp.v(0, ALL, ALL, S(d * 4, d * 4 + 4)), G.v(0, ALL, ALL, S(d * 8 + 4, d * 8 + 8)), AF.Exp, scale=-1.0)
    self.act(sp.v(0), sp.v(0), AF.Ln, bias=self.constcol(1.0))
    pB, pT_ = ps[1], ps[2]
    for blk in range(NBLK):
        self.mm(pB.v(0, ALL, S(blk * 8, blk * 8 + 4)), self.triF, sp.v(0, ALL, blk, S(0, 4)))
        self.mm(pB.v(0, ALL, S(blk * 8 + 4, blk * 8 + 8)), self.triB, sp.v(0, ALL, blk, S(4, 8)))
        self.mm(pT_.v(0, ALL, S(blk * 8, blk * 8 + 8)), self.ones_f, sp.v(0, ALL, blk, ALL))
    bsv = V(Bs.t[:, :, :].rearrange("p a b -> p (a b)"), Bs.v(0).bs)
    btv = V(Bt.t[:, :, :].rearrange("p a b -> p (a b)"), Bt.v(0).bs)
    self.copy(bsv, pB.v(0, ALL, S(0, NBLK * 8)))
    self.copy(btv, pT_.v(0, ALL, S(0, NBLK * 8)))
    for d in range(2):
        self.tt(E1.v(0, ALL, ALL, S(d * 4, d * 4 + 4)), G.v(0, ALL, ALL, S(d * 8, d * 8 + 4)),
                Bs.v(0, ALL, ALL, S(d * 4, d * 4 + 4)), ALU.add)
    self.act(ga.v(0), E1.v(0), AF.Exp, bias=self.constcol(lns))
    self.tt(E1.v(0), E1.v(0), Bt.v(0), ALU.subtract)
    self.act(gc.v(0), E1.v(0), AF.Exp, bias=self.constcol(lns))
    self.act(gib.v(0), Bs.v(0), AF.Exp)
    self.act(gd.v(0), Bt.v(0), AF.Exp, scale=-1.0)
    return ga, gc, gib, gd


def _mlstm_heads(self, l, w_in_d, catM, smal, last, gsc):
    P, ps, hT, pp = self.P, self.ps, self.hT, self.pp
    S, ALL = S_, ALL_
    slots = self.slots
    tob = self.tile_of_blk
    pb = l * NPAR
    ga, gc, gib, gd = gsc
    with ExitStack() as esw:
        rawb = self.sb("rawb", [128, NT + 8], BF16, esw)
        qT = self.sb("mqT", [128, NT], BF16, esw)
        kT = self.sb("mkT", [128, NT], BF16, esw)
        sigT = self.sb("sigT", [128, NT], BF16, esw)
        vaug = self.sb("vaug", [128, NBLK, 130], BF16, esw)
        hsum = self.sb("hsum", [128, NBLK, 128], F32, esw)
        dg = self.sb("dg", [128, 10, 128], BF16, esw)
        sTm = self.sb("sTm", [128, 4, 128], BF16, esw)
        khat = self.sb("khat", [128, 4, 128], BF16, esw)
        Cf = self.sb("Cf", [128, 2, 130], F32, esw)
        Cb = self.sb("Cb", [128, 2, 130], BF16, esw)
        rr = self.sb("rr", [128, 2, 4], F32, esw)
        hn = self.sb("hn", [128, 2, 128], BF16, esw)
        jk = self.sb("jk", [128, 2, 128], F32, esw)
        ssq = self.sb("ssq", [128, 2, 2], F32, esw)
        self.memset(rawb.v(0), 0.0)
        self.memset(vaug.v("ones", ALL, ALL, S(128, 130)), 1.0)
        psK = [V(ps[6].t[:, :].bitcast(BF16), ps[6].v(0).bs), V(ps[7].t[:, :].bitcast(BF16), ps[7].v(0).bs)]
        for h in range(4):
            si = self.slot()
            wv = slots.t[:, si, 0:8 * 512].rearrange("p (k c) -> p k c", c=512)
            wb = slots.v(si).bs
            pairs = []
            for j, base in enumerate((1536, 2048, 2560, 3072)):
                col = base + h * 128
                pairs.append((wv[:, :, j * 128:(j + 1) * 128],
                              w_in_d[l, :, col:col + 128].rearrange("(k p) c -> p k c", p=128)))
            self.dma("pool", V(wv, wb), DV(pairs[0][1], [Buf("d")]), pairs=pairs)
            for w in range(2):
                c = w * 4 + h
                for j in range(5):
                    col = pb + 71 + j * 8 + c
                    self.ts(dg.v(w, ALL, w * 5 + j, ALL), self.ident_bf, pp.v(0, ALL, S(col, col + 1)), ALU.mult)
            for w, dest in ((0, qT), (1, kT)):
                c = w * 4 + h
                for ti, (t0, n) in enumerate(TILES):
                    bank = ps[ti % 2]
                    for k in range(8):
                        self.mm(bank.v(0, ALL, S(0, n)), V(wv[:, k, w * 128:(w + 1) * 128], wb),
                                hT.v((k, ti), ALL, k, S(t0, t0 + n)), start=(k == 0), stop=(k == 7))
                    off = t0 + 2 if ti < 4 else t0 + 6
                    self.copy(rawb.v(0, ALL, S(off, off + n)), bank.v(0, ALL, S(0, n)), eng="act")
                for ti, (t0, n) in enumerate(TILES):
                    bank = ps[2 + ti % 2]
                    off = t0 if ti < 4 else t0 + 4
                    for j in range(5):
                        self.mm(bank.v(0, ALL, S(0, n)), dg.v(w, ALL, w * 5 + j, ALL),
                                rawb.v(0, ALL, S(off + j, off + j + n)), start=(j == 0), stop=(j == 4))
                    self.act(dest.v(0, ALL, S(t0, t0 + n)), bank.v(0, ALL, S(0, n)), AF.Silu,
                             bias=pp.v(0, ALL, S(pb + 111 + c, pb + 112 + c)))
            for g in range(5):
                blks = list(range(g * 4, min(g * 4 + 4, NBLK)))
                bank = ps[4 + g % 2]
                for bi, blk in enumerate(blks):
                    for k in range(8):
                        self.mm(bank.v(0, ALL, S(bi * 128, (bi + 1) * 128)),
                                hT.v((k, tob(blk)), ALL, k, S(blk * 128, (blk + 1) * 128)),
                                V(wv[:, k, 256:384], wb), start=(k == 0), stop=(k == 7))
                nb = len(blks)
                srcv = V(bank.t[:, 0:nb * 128].rearrange("p (a b) -> p a b", b=128), bank.v(0).bs)
                self.copy(vaug.v(0, ALL, S(blks[0], blks[0] + nb), S(0, 128)), srcv, eng="act")
            for ti, (t0, n) in enumerate(TILES):
                if last and ti == 4:
                    continue
                bank = ps[ti % 2]
                for k in range(8):
                    self.mm(bank.v(0, ALL, S(0, n)), V(wv[:, k, 384:512], wb),
                            hT.v((k, ti), ALL, k, S(t0, t0 + n)), start=(k == 0), stop=(k == 7))
                self.act(sigT.v(0, ALL, S(t0, t0 + n)), bank.v(0, ALL, S(0, n)), AF.Sigmoid)
            order = [[16, 17] + list(range(16)), [17, 16] + list(range(15, -1, -1))]
            masks = [self.maskF, self.maskB]
            visited = set()
            npost = 0
            mg = smal.v(8, ALL, S(8 + h, 9 + h))
            for i in range(NBLK):
                for d in range(2):
                    blk = order[d][i]
                    col = d * 4 + h
                    par = i % 2
                    first = i == 0
                    bsl = S(blk * 128, (blk + 1) * 128)
                    pS, pH, pC = ps[d], ps[2 + d], ps[4 + d]
                    kv = kT.v(0, ALL, bsl)
                    qv = qT.v(0, ALL, bsl)
                    need_out = not (last and blk >= 16)
                    if need_out:
                        self.mm(pS.v(0, ALL, S(0, 128)), kv, qv)
                        sv = sTm.v((d, par), ALL, d * 2 + par, ALL)
                        self.stt(sv, pS.v(0, ALL, S(0, 128)), ga.v(0, ALL, blk, S(col, col + 1)), masks[d],
                                 ALU.mult, ALU.mult)
                        self.mm(pH.v(0, ALL, S(0, 129)), sv, vaug.v([0, "ones"], ALL, blk, S(0, 129)),
                                start=True, stop=first)
                        if not first:
                            self.mm(pH.v(0, ALL, S(0, 129)), qv, Cb.v(d, ALL, d, S(0, 129)), start=False, stop=True)
                        r0 = rr.v(d, ALL, d, S(0, 1))
                        r1 = rr.v(d, ALL, d, S(1, 2))
                        self.act(r0, pH.v(0, ALL, S(128, 129)), AF.Abs)
                        self.ts(r0, r0, gib.v(0, ALL, blk, S(col, col + 1)), ALU.max)
                        self.recip(r1, r0)
                        hv = hsum.v(blk, ALL, blk, ALL)
                        if blk not in visited:
                            self.ts(hv, pH.v(0, ALL, S(0, 128)), r1, ALU.mult)
                        else:
                            self.stt(hv, pH.v(0, ALL, S(0, 128)), r1, hv, ALU.mult, ALU.add)
                    if i < NBLK - 1:
                        pk = V(psK[d].ap[:, 0:128], psK[d].bs)
                        self.tr(pk, kv, self.ident_bf)
                        kh = khat.v((d, par), ALL, d * 2 + par, ALL)
                        self.act(kh, pk, AF.Copy, scale=gc.v(0, ALL, blk, S(col, col + 1)))
                        self.mm(pC.v(0, ALL, S(0, 129)), kh, vaug.v([0, "ones"], ALL, blk, S(0, 129)))
                        cfv = Cf.v(d, ALL, d, S(0, 129))
                        if first:
                            self.copy(cfv, pC.v(0, ALL, S(0, 129)))
                        else:
                            self.stt(cfv, cfv, gd.v(0, ALL, blk, S(col, col + 1)), pC.v(0, ALL, S(0, 129)),
                                     ALU.mult, ALU.add)
                        self.copy(Cb.v(d, ALL, d, S(0, 129)), cfv, eng="act")
                    if blk in visited and need_out:
                        npost += 1
                        pq = npost % 2
                        hv = hsum.v(blk, ALL, blk, ALL)
                        jv = jk.v(pq, ALL, pq, ALL)
                        s0 = ssq.v(pq, ALL, pq, S(0, 1))
                        self.act(jv, hv, AF.Square)
                        self.P.add("dve", lambda e, a=s0.ap, b=jv.ap: e.reduce_sum(a, b, AX.X), reads=[jv], writes=[s0])
                        self.rsqrt(s0, s0, float(128 * EPS))
                        hnv = hn.v(pq, ALL, pq, ALL)
                        self.ts(hnv, hv, s0, ALU.mult)
                        pk = V(psK[pq].ap[:, 128:256], psK[pq].bs)
                        self.tr(pk, hnv, self.ident_bf)
                        self.stt(catM.v((h, tob(blk)), ALL, h, bsl), pk, mg, sigT.v(0, ALL, bsl), ALU.mult, ALU.mult)
                    visited.add(blk)


Builder.gates = _gates
Builder.mlstm_heads = _mlstm_heads


def _w_out_half(self, l, w_out_d, half, cat, out_tiles, modv):
    ps, xT = self.ps, self.xT
    S, ALL = S_, ALL_
    si = self.slot()
    wv = self.slots.t[:, si, 0:4096].rearrange("p (k c) -> p k c", c=1024)
    wb = self.slots.v(si).bs
    self.dma("pool", V(wv, wb), DV(w_out_d[l, half * 512:(half + 1) * 512, :].rearrange("(k p) c -> p k c", p=128),
                                   [Buf("d")]))
    cnt = 0
    for m in range(8):
        for ti in out_tiles:
            t0, n = TILES[ti]
            s = 0 if ti < 4 else 1
            bank = ps[cnt % 4]
            cnt += 1
            for k in range(4):
                self.mm(bank.v(0, ALL, S(0, n)), V(wv[:, k, m * 128:(m + 1) * 128], wb),
                        cat.v((k, ti), ALL, k, S(t0, t0 + n)), start=(k == 0), stop=(k == 3))
            xv = xT.v((m, ti), ALL, m, S(t0, t0 + n))
            self.stt(xv, bank.v(0, ALL, S(0, n)), modv(2, m, s), xv, ALU.mult, ALU.add)


Builder.w_out_half = _w_out_half


def _ffn_expert(self, w1_fn, w3_fn, w2_fn, dff, halves, y, tf, gate_v, cb_fn=None, tmpx=None):
    ps, xT, hT = self.ps, self.xT, self.hT
    S, ALL = S_, ALL_
    slots = self.slots
    nch = dff // 128
    for hi, half in enumerate(halves):
        toffs = {}
        o = 0
        for ti in half:
            toffs[ti] = o
            o += TILES[ti][1]
        cb = cb_fn(hi, half, toffs) if cb_fn is not None else None
        for c0 in range(0, nch, 8):
            c1 = min(c0 + 8, nch)
            if getattr(self, "dbg_maxg", None) is not None and (c0 // 8 >= self.dbg_maxg or hi >= 1):
                continue
            for b0 in range(c0, c1, 4):
                b1 = min(b0 + 4, c1)
                ncol = (b1 - b0) * 128
                si = self.slot()
                wv = slots.t[:, si, 0:8192].rearrange("p (w k c) -> p w k c", w=2, k=8)
                wb = slots.v(si).bs
                pairs = [(wv[:, 0, :, 0:ncol], w1_fn(b0 * 128, ncol)), (wv[:, 1, :, 0:ncol], w3_fn(b0 * 128, ncol))]
                self.dma("pool", V(wv, wb), DV(pairs[0][1], [Buf("d")]), pairs=pairs)
                for j in range(b0, b1):
                    jj = j - b0
                    for ti in half:
                        t0, n = TILES[ti]
                        self.cntA += 1
                        par = self.cntA % 2
                        bU, bV = ps[par], ps[2 + par]
                        for k in range(8):
                            self.mm(bU.v(0, ALL, S(0, n)), V(wv[:, 0, k, jj * 128:(jj + 1) * 128], wb),
                                    hT.v((k, ti), ALL, k, S(t0, t0 + n)), start=(k == 0), stop=(k == 7))
                        for k in range(8):
                            self.mm(bV.v(0, ALL, S(0, n)), V(wv[:, 1, k, jj * 128:(jj + 1) * 128], wb),
                                    hT.v((k, ti), ALL, k, S(t0, t0 + n)), start=(k == 0), stop=(k == 7))
                        sv = tf.v(par, ALL, par, S(0, n))
                        self.act(sv, bU.v(0, ALL, S(0, n)), AF.Silu)
                        self.tt(y.v((j - c0, toffs[ti]), ALL, j - c0, S(toffs[ti], toffs[ti] + n)), sv,
                                bV.v(0, ALL, S(0, n)), ALU.mult)
            si = self.slot()
            ng = c1 - c0
            wv = slots.t[:, si, 0:ng * 1024].rearrange("p (k c) -> p k c", c=1024)
            wb = slots.v(si).bs
            self.dma("pool", V(wv, wb), DV(w2_fn(c0 * 128, ng * 128), [Buf("d")]))
            for m in range(8):
                for ti in half:
                    t0, n = TILES[ti]
                    s = 0 if ti < 4 else 1
                    self.cntB += 1
                    bO = ps[4 + self.cntB % 2]
                    for jj in range(ng):
                        self.mm(bO.v(0, ALL, S(0, n)), V(wv[:, jj, m * 128:(m + 1) * 128], wb),
                                y.v((jj, toffs[ti]), ALL, jj, S(toffs[ti], toffs[ti] + n)), start=(jj == 0), stop=(jj == ng - 1))
                    xv = xT.v((m, ti), ALL, m, S(t0, t0 + n))
                    if cb is None:
                        self.stt(xv, bO.v(0, ALL, S(0, n)), gate_v(m, s), xv, ALU.mult, ALU.add)
                    else:
                        tv = tmpx.v(self.cntB % 2, ALL, self.cntB % 2, S(0, n))
                        self.stt(tv, bO.v(0, ALL, S(0, n)), gate_v(m, s), cb.v(toffs[ti], ALL, S(toffs[ti], toffs[ti] + n)),
                                 ALU.mult, ALU.mult)
                        self.tt(xv, xv, tv, ALU.add)


def _ffn_dense(self, l, w1_d, w3_d, w2_d, modv):
    S, ALL = S_, ALL_
    j = l // 2
    with ExitStack() as esf:
        self.slots = self.sb("fslot", [128, 3, 8192], BF16, esf)
        self.nslots = 3
        self.slot_i = 0
        y = self.sb("y", [128, 8, 1280], BF16, esf)
        tf = self.sb("ftf", [128, 2, 512], F32, esf)
        self.cntA = 0
        self.cntB = 0

        def w1_fn(c0, nc_):
            return w1_d[j, :, c0:c0 + nc_].rearrange("(k p) c -> p k c", p=128)

        def w3_fn(c0, nc_):
            return w3_d[j, :, c0:c0 + nc_].rearrange("(k p) c -> p k c", p=128)

        def w2_fn(r0, nr):
            return w2_d[j, r0:r0 + nr, :].rearrange("(k p) c -> p k c", p=128)

        if self.stage == l * 10 + 8:
            self.dbg_maxg = 1
        self.ffn_expert(w1_fn, w3_fn, w2_fn, D_FF, [[0, 1], [2, 3, 4]], y, tf, lambda m, s: modv(5, m, s))
        if self.stage == l * 10 + 8:
            return self.finish_debug({"y": (y, BF16, [128, 8, 1280]), "tf": (tf, F32, [128, 2, 512]),
                                      "xT": (self.xT, F32, [128, 8, NT]), "slots": (self.slots, BF16, [128, 3, 8192])})
        return None


Builder.ffn_expert = _ffn_expert
Builder.ffn_dense = _ffn_dense


def _moe(self, l, router_d, w1_d, w3_d, w2_d, ABv, modv):
    P, ps, hT = self.P, self.ps, self.hT
    S, ALL = S_, ALL_
    j = l // 2
    LT = TILES[:4]
    with ExitStack() as esf:
        wr = self.sb("wr", [128, 8, 8], F32, esf)
        combT = self.sb("combT", [8, SEQ], BF16, esf)
        self.dma("sp", wr.v(0), DV(router_d[j, :, :].rearrange("(k p) e -> p k e", p=128), [Buf("d")]))
        pR = ps[7]
        self.norm_mod(1, LT, ABv, modv, 3, router=(wr, pR))
        if self.stage == l * 10 + 6:
            lgd = self.sb("lgd", [128, 128], F32, esf)
            self.copy(lgd.v(0), pR.v(0, ALL, S(0, 128)))
            return self.finish_debug({"hT": (hT, BF16, [128, 8, NT]), "lg": (lgd, F32, [128, 128])})
        with ExitStack() as est:
            lg = self.sb("lg", [128, 16, 8], F32, est)
            l2 = self.sb("l2", [128, 16, 8], F32, est)
            mk1 = self.sb("mk1", [128, 16, 8], F32, est)
            mk2 = self.sb("mk2", [128, 16, 8], F32, est)
            m1 = self.sb("m1", [128, 16], F32, est)
            m2 = self.sb("m2", [128, 16], F32, est)
            w1 = self.sb("w1", [128, 16], F32, est)
            w2 = self.sb("w2", [128, 16], F32, est)
            comb = self.sb("comb", [128, 16, 8], BF16, est)

            def bc(tn):
                return V(tn.t[:, :].unsqueeze(2).to_broadcast([128, 16, 8]), tn.v(0).bs)

            lgf = V(lg.t[:, :, :].rearrange("p a b -> p (a b)"), lg.v(0).bs)
            self.copy(lgf, pR.v(0, ALL, S(0, 128)))
            self.rmax(m1.v(0), lg.v(0))
            self.tt(mk1.v(0), lg.v(0), bc(m1), ALU.is_equal)
            self.stt(l2.v(0), mk1.v(0), -1e30, lg.v(0), ALU.mult, ALU.add)
            self.rmax(m2.v(0), l2.v(0))
            self.tt(mk2.v(0), l2.v(0), bc(m2), ALU.is_equal)
            self.tt(w2.v(0), m2.v(0), m1.v(0), ALU.subtract)
            self.act(w2.v(0), w2.v(0), AF.Sigmoid)
            self.ts(w1.v(0), w2.v(0), -1.0, ALU.mult, 1.0, ALU.add)
            self.tt(mk1.v(0), mk1.v(0), bc(w1), ALU.mult)
            self.tt(mk2.v(0), mk2.v(0), bc(w2), ALU.mult)
            self.tt(comb.v(0), mk1.v(0), mk2.v(0), ALU.add)
            for g in range(2):
                bank = ps[g]
                pk = bank.t[:, :].bitcast(BF16)
                for bi in range(8):
                    blk = g * 8 + bi
                    self.tr(V(pk[0:8, bi * 128:(bi + 1) * 128], bank.v(0).bs), comb.v(0, ALL, blk, ALL), self.ident_bf)
                self.copy(combT.v(0, ALL, S(g * 1024, (g + 1) * 1024)), V(pk[0:8, 0:1024], bank.v(0).bs))
            if self.stage == l * 10 + 8:
                return self.finish_debug({"comb": (comb, BF16, [128, 16, 8]), "combT": (combT, BF16, [8, SEQ])})
        P.barrier()
        self.slots = self.sb("fslot", [128, 3, 8192], BF16, esf)
        self.nslots = 3
        self.slot_i = 0
        y = self.sb("y", [128, 8, 1024], BF16, esf)
        tf = self.sb("ftf", [128, 2, 512], F32, esf)
        tmpx = self.sb("tmpx", [128, 2, 512], F32, esf)
        cbt = self.sb("cbt", [128, 1024], F32, esf)
        self.cntA = 0
        self.cntB = 0
        nexp = NEXP if getattr(self, "dbg_nexp", None) is None else self.dbg_nexp
        for e in range(nexp):
            def w1_fn(c0, nc_, e=e):
                return w1_d[j, e, :, c0:c0 + nc_].rearrange("(k p) c -> p k c", p=128)

            def w3_fn(c0, nc_, e=e):
                return w3_d[j, e, :, c0:c0 + nc_].rearrange("(k p) c -> p k c", p=128)

            def w2_fn(r0, nr, e=e):
                return w2_d[j, e, r0:r0 + nr, :].rearrange("(k p) c -> p k c", p=128)

            def cb_fn(hi, half, toffs, e=e):
                for ti in half:
                    t0, n = TILES[ti]
                    self.mm(ps[6].v(0, ALL, S(0, n)), self.sel.v(0, ALL, S(e * 128, (e + 1) * 128)),
                            combT.v(0, ALL, S(t0, t0 + n)))
                    self.copy(cbt.v(toffs[ti], ALL, S(toffs[ti], toffs[ti] + n)), ps[6].v(0, ALL, S(0, n)), eng="act")
                return cbt

            self.ffn_expert(w1_fn, w3_fn, w2_fn, D_FFE, [[0, 1], [2, 3]], y, tf, lambda m, s: modv(5, m, s),
                            cb_fn=cb_fn, tmpx=tmpx)
    return None


Builder.moe = _moe


def _moe_sparse(self, l, router_d, ABv, modv):
    P, ps, xT = self.P, self.ps, self.xT
    S, ALL = S_, ALL_
    I32 = mybir.dt.int32
    NTILE = 15
    T = 512
    m1_d = [self.din(f"m1_{b}", [NEXP, D, 256]) for b in range(14)]
    m3_d = [self.din(f"m3_{b}", [NEXP, D, 256]) for b in range(14)]
    m2_d = [[self.din(f"m2_{rp}_{dh}", [NEXP, 512, 512]) for dh in range(2)] for rp in range(7)]
    iota_d = self.din("iota", [128, 512])
    with ExitStack() as esf:
        h2tok = self.sb("h2tok", [128, 16, D], BF16, esf)
        iota_t = self.sb("iota", [128, 512], F32, esf)
        sl1 = self.sb("sl1", [128, 16], F32, esf)
        sl2 = self.sb("sl2", [128, 16], F32, esf)
        w2t = self.sb("w2t", [128, 16], F32, esf)
        dwt = self.sb("dwt", [128, 16], F32, esf)
        esi = self.sb("esi", [128, 16], I32, esf)
        self.dma("sp", iota_t.v(0), DV(iota_d[:, :], [Buf("d")]))
        with ExitStack() as esr:
            wr = self.sb("wr", [128, 8, 8], F32, esr)
            h2f = self.sb("h2f", [128, 8, 512], BF16, esr)
            self.dma("sp", wr.v(0), DV(router_d[l // 2, :, :].rearrange("(k p) e -> p k e", p=128), [Buf("d")]))
            pR = ps[7]

            def dst(c, ti, n):
                return h2f.v(c, ALL, c, S(0, n))

            def after_tile(ti):
                for bi in range(4):
                    bank = ps[2 + (ti * 4 + bi) % 4]
                    pk = bank.t[:, :].bitcast(BF16)
                    for c in range(8):
                        self.tr(V(pk[:, c * 128:(c + 1) * 128], bank.v(0).bs), h2f.v(c, ALL, c, S(bi * 128, (bi + 1) * 128)),
                                self.ident_bf)
                    self.copy(h2tok.v(ti * 4 + bi, ALL, ti * 4 + bi, ALL), V(pk[:, 0:1024], bank.v(0).bs),
                              eng="act" if bi % 2 == 0 else "dve")

            self.norm_mod(1, TILES[:4], ABv, modv, 3, router=(wr, pR), dst=dst, after_tile=after_tile)
            lg = self.sb("lg", [128, 16, 8], F32, esr)
            l2 = self.sb("l2", [128, 16, 8], F32, esr)
            mk1 = self.sb("mk1", [128, 16, 8], F32, esr)
            mk2 = self.sb("mk2", [128, 16, 8], F32, esr)
            Mm = self.sb("Mm", [128, 16, 8], F32, esr)
            rank = self.sb("rank", [128, 16, 8], F32, esr)
            tot = self.sb("tot", [128, 16, 8], F32, esr)
            pbk = self.sb("pbk", [128, 16, 8], F32, esr)
            m1 = self.sb("m1", [128, 16], F32, esr)
            m2 = self.sb("m2", [128, 16], F32, esr)
            w1t = self.sb("w1t", [128, 16], F32, esr)
            triS = self.sb("triS", [128, 128], F32, esr)
            sm8 = self.sb("sm8", [128, 8, 8], F32, esr)
            esf_ = self.sb("esf", [128, 16], F32, esr)

            def bc(tn):
                return V(tn.t[:, :].unsqueeze(2).to_broadcast([128, 16, 8]), tn.v(0).bs)

            def flat(tn):
                return V(tn.t[:, :, :].rearrange("p a b -> p (a b)"), tn.v(0).bs)

            self.copy(flat(lg), pR.v(0, ALL, S(0, 128)))
            self.rmax(m1.v(0), lg.v(0))
            self.tt(mk1.v(0), lg.v(0), bc(m1), ALU.is_equal)
            self.stt(l2.v(0), mk1.v(0), -1e30, lg.v(0), ALU.mult, ALU.add)
            self.rmax(m2.v(0), l2.v(0))
            self.tt(mk2.v(0), l2.v(0), bc(m2), ALU.is_equal)
            self.tt(w2t.v(0), m2.v(0), m1.v(0), ALU.subtract)
            self.act(w2t.v(0), w2t.v(0), AF.Sigmoid)
            self.ts(w1t.v(0), w2t.v(0), -1.0, ALU.mult, 1.0, ALU.add)
            self.tt(dwt.v(0), w1t.v(0), w2t.v(0), ALU.subtract)
            self.tt(Mm.v(0), mk1.v(0), mk2.v(0), ALU.add)
            self.tt(triS.v(0), self.triF, self.ident_f, ALU.subtract)
            self.mm(ps[0].v(0, ALL, S(0, 128)), triS.v(0), flat(Mm))
            self.mm(ps[1].v(0, ALL, S(0, 128)), self.ones_f, flat(Mm))
            self.copy(flat(rank), ps[0].v(0, ALL, S(0, 128)))
            self.copy(flat(tot), ps[1].v(0, ALL, S(0, 128)))
            self.memset(pbk.v(0, ALL, 0, ALL), 0.0)
            for b in range(1, 16):
                self.tt(pbk.v(0, ALL, b, ALL), pbk.v(0, ALL, b - 1, ALL), tot.v(0, ALL, b - 1, ALL), ALU.add)

            def row(i):
                return sm8.v(0, ALL, i, ALL)

            ntot, ntl, st, end, offs, tmp8 = [row(i) for i in range(6)]
            self.tt(ntot, pbk.v(0, ALL, 15, ALL), tot.v(0, ALL, 15, ALL), ALU.add)
            self.ts(ntl, ntot, 0.0, ALU.is_gt)
            for k in range(1, 4):
                self.stt(ntl, ntot, float(T * k), ntl, ALU.is_gt, ALU.add)
            self.memset(sm8.v(0, ALL, 2, S(0, 1)), 0.0)
            for e in range(1, 8):
                self.tt(sm8.v(0, ALL, 2, S(e, e + 1)), sm8.v(0, ALL, 2, S(e - 1, e)), sm8.v(0, ALL, 1, S(e - 1, e)), ALU.add)
            self.tt(end, st, ntl, ALU.add)
            self.ts(offs, st, float(T), ALU.mult)
            self.tt(rank.v(0), rank.v(0), pbk.v(0), ALU.add)
            offs_b = V(sm8.t[:, 4, :].unsqueeze(1).to_broadcast([128, 16, 8]), sm8.v(0).bs)
            self.tt(rank.v(0), rank.v(0), offs_b, ALU.add)
            self.tt(l2.v(0), mk1.v(0), rank.v(0), ALU.mult)
            self.P.add("dve", lambda e: e.reduce_sum(sl1.t[:, :], l2.t[:, :, :], AX.X), reads=[l2.v(0)], writes=[sl1.v(0)])
            self.tt(lg.v(0), mk2.v(0), rank.v(0), ALU.mult)
            self.P.add("dve", lambda e: e.reduce_sum(sl2.t[:, :], lg.t[:, :, :], AX.X), reads=[lg.v(0)], writes=[sl2.v(0)])
            self.memset(esf_.v(0), 0.0)
            for s in range(NTILE):
                self.ts(tmp8, end, float(s), ALU.is_le)
                self.P.add("dve", lambda e, a=esf_.t[:, s:s + 1], b=sm8.t[:, 5, :]: e.reduce_sum(a, b, AX.X),
                           reads=[tmp8], writes=[esf_.v(0)])
            self.ts(esf_.v(0), esf_.v(0), 7.0, ALU.min)
            self.copy(esi.v(0), esf_.v(0))
        P.barrier()
        self.slots = self.sb("fslot", [128, 3, 4096], BF16, esf)
        self.nslots = 3
        self.slot_i = 0
        hs = self.sb("hs", [128, 8, T], BF16, esf)
        y = self.sb("ysp", [128, 28, T], BF16, esf)
        otok = self.sb("otok", [128, 4, D], BF16, esf)
        swt = self.sb("swt", [128, 2, 4, T], BF16, esf)
        a1 = self.sb("a1", [128, 2, T], BF16, esf)
        selb = self.sb("selb", [128, 2, T], BF16, esf)
        tmpw = self.sb("tmpw", [128, 2, T], BF16, esf)
        selw = self.sb("selw", [128, 2, T], BF16, esf)
        tf = self.sb("stf", [128, 2, T], BF16, esf)
        s1s = self.sb("s1s", [128, 2, 16], F32, esf)
        s2s = self.sb("s2s", [128, 2, 16], F32, esf)
        n1s = self.sb("n1s", [128, 2, 16], F32, esf)
        n2s = self.sb("n2s", [128, 2, 16], F32, esf)
        adf = self.sb("adf", [128, 2, T], BF16, esf)
        adf2 = self.sb("adf2", [128, 2, T], BF16, esf)
        w1p = self.sb("w1p", [128, 16], F32, esf)
        w1n = self.sb("w1n", [128, 16], F32, esf)
        w2n = self.sb("w2n", [128, 16], F32, esf)
        self.tt(w1p.v(0), w2t.v(0), dwt.v(0), ALU.add)
        self.ts(w1n.v(0), w1p.v(0), -1.0, ALU.mult)
        self.ts(w2n.v(0), w2t.v(0), -1.0, ALU.mult)
        iota = iota_t.v(0)
        cnt = 0
        ntiles = NTILE if getattr(self, "dbg_ntiles", None) is None else self.dbg_ntiles
        for s in range(ntiles):
            sp_ = s % 2
            holder = {}

            def load_reg(e, s=s, holder=holder):
                r = e.alloc_register(f"ex{s}")
                ins = e.reg_load(r, esi.t[0:1, s:s + 1])
                holder["v"] = e.snap(r, donate=True, min_val=0, max_val=NEXP - 1)
                return ins

            P.add("pool", load_reg, reads=[esi.v(0)])
            self.ts(s1s.v(sp_, ALL, sp_, ALL), sl1.v(0), float(-T * s), ALU.add)
            self.ts(s2s.v(sp_, ALL, sp_, ALL), sl2.v(0), float(-T * s), ALU.add)
            self.ts(n1s.v(sp_, ALL, sp_, ALL), sl1.v(0), -1.0, ALU.mult, float(T * s), ALU.add)
            self.ts(n2s.v(sp_, ALL, sp_, ALL), sl2.v(0), -1.0, ALU.mult, float(T * s), ALU.add)

            def build_sel(tb, dstv, eng="dve"):
                av = a1.v(tb % 2, ALL, tb % 2, ALL)
                dv_ = adf.v(tb % 2, ALL, tb % 2, ALL)
                self.act(dv_, iota, AF.Abs, bias=n1s.v(sp_, ALL, sp_, S(tb, tb + 1)))
                self.act(av, dv_, AF.Relu, bias=self.constcol(1.0), scale=-1.0)
                self.stt(dstv, iota, s2s.v(sp_, ALL, sp_, S(tb, tb + 1)), av, ALU.is_equal, ALU.add)
                return av

            def build_selw(tb, dstv):
                q2 = tb % 2
                d1 = adf.v(q2, ALL, q2, ALL)
                t1 = a1.v(q2, ALL, q2, ALL)
                self.act(d1, iota, AF.Abs, bias=n1s.v(sp_, ALL, sp_, S(tb, tb + 1)))
                self.act(t1, d1, AF.Relu, bias=w1p.v(0, ALL, S(tb, tb + 1)), scale=w1n.v(0, ALL, S(tb, tb + 1)))
                t2 = tmpw.v(q2, ALL, q2, ALL)
                self.ts(t2, iota, s2s.v(sp_, ALL, sp_, S(tb, tb + 1)), ALU.is_equal, w2t.v(0, ALL, S(tb, tb + 1)), ALU.mult)
                self.tt(dstv, t1, t2, ALU.add)

            for tb in range(16):
                sv = selb.v(tb % 2, ALL, tb % 2, ALL)
                build_sel(tb, sv, eng=SEL_ENG)
                for c in range(8):
                    self.mm(ps[c].v(0), h2tok.v(tb, ALL, tb, S(c * 128, (c + 1) * 128)), sv,
                            start=(tb == 0), stop=(tb == 15))
            for c in range(8):
                self.copy(hs.v(c, ALL, c, ALL), ps[c].v(0), eng="act")
            for b in range(14):
                si = self.slot()
                wv = self.slots.t[:, si, 0:4096].rearrange("p (w k c) -> p w k c", w=2, k=8)
                wb = self.slots.v(si).bs

                def dfn(e, b=b, wv=wv, holder=holder):
                    v = holder["v"]
                    i1 = m1_d[b][bass.ds(v, 1), :, :].rearrange("e (k p) c -> p (e k) c", p=128)
                    i3 = m3_d[b][bass.ds(v, 1), :, :].rearrange("e (k p) c -> p (e k) c", p=128)
                    return [e.dma_start(out=wv[:, 0, :, :], in_=i1), e.dma_start(out=wv[:, 1, :, :], in_=i3)]

                P.add("pool", dfn, reads=[DV(None, [Buf("d")])], writes=[V(wv, wb)], dma=2, sembuf=wb[0])
                for jj in range(2):
                    jc = b * 2 + jj
                    cnt += 1
                    par = cnt % 2
                    bU, bV = ps[par], ps[2 + par]
                    for k in range(8):
                        self.mm(bU.v(0), V(wv[:, 0, k, jj * 128:(jj + 1) * 128], wb), hs.v(k, ALL, k, ALL),
                                start=(k == 0), stop=(k == 7))
                    for k in range(8):
                        self.mm(bV.v(0), V(wv[:, 1, k, jj * 128:(jj + 1) * 128], wb), hs.v(k, ALL, k, ALL),
                                start=(k == 0), stop=(k == 7))
                    sv = tf.v(par, ALL, par, ALL)
                    self.act(sv, bU.v(0), AF.Silu)
                    self.tt(y.v(jc, ALL, jc, ALL), sv, bV.v(0), ALU.mult)
            for dh in range(2):
                for rp in range(7):
                    si = self.slot()
                    wv = self.slots.t[:, si, 0:2048].rearrange("p (k c) -> p k c", c=512)
                    wb = self.slots.v(si).bs

                    def dfn2(e, rp=rp, dh=dh, wv=wv, holder=holder):
                        v = holder["v"]
                        i2 = m2_d[rp][dh][bass.ds(v, 1), :, :].rearrange("e (k p) c -> p (e k) c", p=128)
                        return [e.dma_start(out=wv, in_=i2)]

                    P.add("pool", dfn2, reads=[DV(None, [Buf("d")])], writes=[V(wv, wb)], dma=1, sembuf=wb[0])
                    for jj in range(4):
                        jc = rp * 4 + jj
                        for sb in range(4):
                            self.mm(ps[4 + sb].v(0), y.v(jc, ALL, jc, S(sb * 128, (sb + 1) * 128)),
                                    V(wv[:, jj, :], wb), start=(jc == 0), stop=(jc == 27))
                for sb in range(4):
                    self.copy(otok.v((sb, dh), ALL, sb, S(dh * 512, (dh + 1) * 512)), ps[4 + sb].v(0), eng="act")
            def stageX(tt_):
                q2 = tt_ % 2
                banks = [ps[0], ps[1]] if q2 == 0 else [ps[4], ps[5]]
                pks = [bk.t[:, :].bitcast(BF16) for bk in banks]
                for tbi in range(4):
                    tb = tt_ * 4 + tbi
                    swv = selw.v(tb % 2, ALL, tb % 2, ALL)
                    build_selw(tb, swv)
                    for sb in range(4):
                        col = (sb % 2) * 512 + tbi * 128
                        self.tr(V(pks[sb // 2][:, col:col + 128], banks[sb // 2].v(0).bs),
                                selw.v(tb % 2, ALL, tb % 2, S(sb * 128, (sb + 1) * 128)), self.ident_bf)
                for hb in range(2):
                    self.copy(V(swt.t[:, q2, 2 * hb:2 * hb + 2, :].rearrange("p a b -> p (a b)"), swt.v((q2, hb)).bs),
                              V(pks[hb][:, 0:1024], banks[hb].v(0).bs), eng="act")

            def stageY(tt_):
                nonlocal cnt
                q2 = tt_ % 2
                t0 = tt_ * 512
                for c in range(8):
                    cnt += 1
                    bD = ps[2 + cnt % 2]
                    for sb in range(4):
                        self.mm(bD.v(0), otok.v([(sb, 0), (sb, 1)], ALL, sb, S(c * 128, (c + 1) * 128)),
                                swt.v((q2, sb // 2), ALL, q2, sb, ALL), start=(sb == 0), stop=(sb == 3))
                    xv = xT.v((c, tt_), ALL, c, S(t0, t0 + 512))
                    self.stt(xv, bD.v(0), modv(5, c, 0), xv, ALU.mult, ALU.add)

            stageX(0)
            for tt_ in range(4):
                if tt_ + 1 < 4:
                    stageX(tt_ + 1)
                stageY(tt_)
    return None


Builder.moe_sparse = _moe_sparse
```

```python
import numpy as np
from contextlib import ExitStack
import concourse.bass as bass
import concourse.mybir as mybir
from concourse.bass_utils import run_bass_kernel_spmd

F32 = mybir.dt.float32
BF16 = mybir.dt.bfloat16
AF = mybir.ActivationFunctionType
ALU = mybir.AluOpType
AX = mybir.AxisListType

D = 1024
SEQ = 2048
CTX = 256
NT = SEQ + CTX
DEPTH = 2
A_DH = 64
IN_COLS = 3600
D_FF = 2816
D_FFE = 3584
NEXP = 8
EPS = 1e-6
NPAR = 391
SPARSE_MOE = True
SEL_ENG = "dve"
TILES = [(0, 512), (512, 512), (1024, 512), (1536, 512), (2048, 256)]
NBLK = NT // 128


class Buf:
    __slots__ = ("name", "lw", "rd", "rd_dma", "sem", "semcnt")

    def __init__(self, name):
        self.name = name
        self.lw = None
        self.rd = {}
        self.rd_dma = []
        self.sem = None
        self.semcnt = 0


class V:
    __slots__ = ("ap", "bs")

    def __init__(self, ap, bs):
        self.ap = ap
        self.bs = bs


class DV(V):
    __slots__ = ()


class LazyDram:
    def __init__(self, bld, name, shape, dt):
        self.bld, self.name, self.shape, self.dt = bld, name, shape, dt
        self.t = None

    def __getitem__(self, key):
        if self.t is None:
            self.t = self.bld.nc.dram_tensor(self.name, list(self.shape), self.dt, kind="ExternalInput").ap()
            self.bld.dram[self.name] = self.t
        return self.t[key]


class Tn:
    def __init__(self, t, name):
        self.t = t
        self.name = name
        self.bufs = {}

    def v(self, key, *sl):
        ks = key if isinstance(key, list) else [key]
        bs = []
        for k in ks:
            b = self.bufs.get(k)
            if b is None:
                b = self.bufs[k] = Buf(f"{self.name}/{k}")
            bs.append(b)
        ap = self.t[sl] if sl else self.t[:]
        return V(ap, bs)


class Op:
    __slots__ = ("eng", "fn", "deps", "ddeps", "dma", "sem", "val", "waits", "sig", "need")

    def __init__(self, eng, fn):
        self.eng = eng
        self.fn = fn
        self.deps = {}
        self.ddeps = set()
        self.dma = 0
        self.sem = None
        self.val = 0
        self.waits = []
        self.sig = None
        self.need = False


class Prog:
    MAXV = 20000

    def __init__(self, nc, es):
        self.nc = nc
        self.es = es
        self.ops = []
        self.engs = {"pe": nc.tensor, "act": nc.scalar, "dve": nc.vector, "pool": nc.gpsimd, "sp": nc.sync}
        self.last_c = {}
        self.dma_all = []
        self.nsem = 0

    def new_sem(self):
        self.nsem += 1
        return self.es.enter_context(self.nc.semaphore(f"s{self.nsem}"))

    def add(self, eng, fn, reads=(), writes=(), dma=0, sembuf=None):
        idx = len(self.ops)
        op = Op(eng, fn)
        rb = [b for v in reads for b in v.bs]
        wb = [b for v in writes for b in v.bs]

        def dep(j):
            o = self.ops[j]
            if o.dma:
                op.ddeps.add(j)
            else:
                if op.deps.get(o.eng, -1) < j:
                    op.deps[o.eng] = j

        for b in rb:
            if b.lw is not None:
                dep(b.lw)
        for b in wb:
            if b.lw is not None:
                dep(b.lw)
            for j in b.rd.values():
                dep(j)
            for j in b.rd_dma:
                dep(j)
        for b in rb:
            if dma:
                b.rd_dma.append(idx)
            else:
                b.rd[eng] = idx
        for b in wb:
            b.lw = idx
            b.rd = {}
            b.rd_dma = []
        if dma:
            op.dma = dma
            if sembuf.sem is None:
                sembuf.sem = self.new_sem()
            sembuf.semcnt += 16 * dma
            op.sem = sembuf.sem
            op.val = sembuf.semcnt
            self.dma_all.append(idx)
        self.ops.append(op)
        if not dma and fn is not None:
            self.last_c[eng] = idx
        return idx

    def barrier(self):
        lasts = dict(self.last_c)
        dmas = list(self.dma_all)
        self.dma_all = []
        for e in ("pe", "act", "dve", "pool", "sp"):
            op = Op(e, None)
            op.deps = dict(lasts)
            op.ddeps = set(dmas)
            self.ops.append(op)

    def emit(self):
        ops = self.ops
        engs = ("pe", "act", "dve", "pool", "sp")
        waited = {e: {f: -1 for f in engs} for e in engs}
        waited_d = {e: {} for e in engs}
        for i, op in enumerate(ops):
            e = op.eng
            for f, j in op.deps.items():
                if f == "pe" and e == "pe":
                    continue
                if waited[e][f] >= j:
                    continue
                waited[e][f] = j
                ops[j].need = True
                op.waits.append(("c", j))
            for j in op.ddeps:
                o = ops[j]
                if waited_d[e].get(o.sem, 0) >= o.val:
                    continue
                waited_d[e][o.sem] = o.val
                op.waits.append(("d", j))
        cnt = {e: 0 for e in engs}
        sems = {e: [] for e in engs}
        for op in ops:
            if op.need:
                cnt[op.eng] += 1
                n = cnt[op.eng]
                k = (n - 1) // self.MAXV
                while len(sems[op.eng]) <= k:
                    sems[op.eng].append(self.new_sem())
                op.sig = (sems[op.eng][k], n - k * self.MAXV)
        for op in ops:
            eng = self.engs[op.eng]
            for kind, j in op.waits:
                o = ops[j]
                if kind == "c":
                    eng.wait_ge(o.sig[0], o.sig[1])
                else:
                    eng.wait_ge(o.sem, o.val)
            if op.fn is None:
                continue
            ins = op.fn(eng)
            if op.dma:
                if not isinstance(ins, (list, tuple)):
                    ins = [ins]
                assert len(ins) == op.dma
                for x in ins:
                    x.then_inc(op.sem, 16)
            elif op.need:
                ins.then_inc(op.sig[0], 1)
        return cnt


class Builder:
    def __init__(self, stage=99, dbg=False):
        self.stage = stage
        self.dbg = dbg
        self.nc = nc = bass.Bass("TRN2", target_bir_lowering=False)
        self.es = ExitStack()
        self.P = Prog(nc, self.es)
        self.dram = {}
        self.uid = 0
        self.ccols = {}
        self.cct = self.sb("cct", [128, 16], F32)

    def din(self, name, shape, dt=F32):
        return LazyDram(self, name, shape, dt)

    def dout(self, name, shape, dt=F32):
        t = self.nc.dram_tensor(name, list(shape), dt, kind="ExternalOutput").ap()
        self.dram[name] = t
        return t

    def sb(self, name, shape, dt, es=None):
        self.uid += 1
        t = (es or self.es).enter_context(self.nc.sbuf_tensor(f"{name}_{self.uid}", list(shape), dt))
        return Tn(t, name)

    def mm(self, out, lhsT, rhs, start=True, stop=True, skip=False):
        if skip:
            self.P.add("pe", lambda e: e.matmul(out.ap, lhsT.ap, rhs.ap, start=start, stop=stop, skip_group_check=True),
                       reads=[lhsT, rhs], writes=[out])
        else:
            self.P.add("pe", lambda e: e.matmul(out.ap, lhsT.ap, rhs.ap, start=start, stop=stop),
                       reads=[lhsT, rhs], writes=[out])

    def tr(self, out, in_, ident):
        self.P.add("pe", lambda e: e.transpose(out.ap, in_.ap, ident.ap), reads=[in_, ident], writes=[out])

    def act(self, out, in_, func, bias=None, scale=None, accum=None):
        kw = {}
        rd = [in_]
        wr = [out]
        if bias is not None:
            if isinstance(bias, V):
                kw["bias"] = bias.ap
                rd.append(bias)
            else:
                kw["bias"] = bias
        if scale is not None:
            if isinstance(scale, V):
                kw["scale"] = scale.ap
                rd.append(scale)
            else:
                kw["scale"] = scale
        if accum is not None:
            kw["accum_out"] = accum.ap
            wr.append(accum)
        self.P.add("act", lambda e: e.activation(out.ap, in_.ap, func, **kw), reads=rd, writes=wr)

    def tt(self, out, in0, in1, op, eng="dve"):
        self.P.add(eng, lambda e: e.tensor_tensor(out.ap, in0.ap, in1.ap, op), reads=[in0, in1], writes=[out])

    def ts(self, out, in0, s1, op0, s2=None, op1=None, eng="dve", accum=None):
        rd = [in0]
        wr = [out]
        a1 = s1
        a2 = s2
        if isinstance(s1, V):
            rd.append(s1)
            a1 = s1.ap
        if isinstance(s2, V):
            rd.append(s2)
            a2 = s2.ap
        kw = {}
        if op1 is not None:
            kw["op1"] = op1
        if accum is not None:
            kw["accum_out"] = accum.ap
            wr.append(accum)
        self.P.add(eng, lambda e: e.tensor_scalar(out.ap, in0.ap, a1, a2, op0, **kw), reads=rd, writes=wr)

    def stt(self, out, in0, scalar, in1, op0, op1, eng="dve"):
        rd = [in0, in1]
        a = scalar
        if isinstance(scalar, V):
            rd.append(scalar)
            a = scalar.ap
        self.P.add(eng, lambda e: e.scalar_tensor_tensor(out.ap, in0.ap, a, in1.ap, op0, op1), reads=rd, writes=[out])

    def copy(self, out, in_, eng="dve"):
        if eng == "act":
            self.P.add("act", lambda e: e.copy(out.ap, in_.ap), reads=[in_], writes=[out])
        else:
            self.P.add(eng, lambda e: e.tensor_copy(out.ap, in_.ap), reads=[in_], writes=[out])

    def rsqrt(self, out, in_, addc):
        self.act(out, in_, AF.Ln, bias=self.constcol(addc))
        self.act(out, out, AF.Exp, scale=-0.5)

    def constcol(self, val):
        key = float(val)
        if key not in self.ccols:
            i = len(self.ccols)
            v = self.cct.v(i, slice(None), slice(i, i + 1))
            self.memset(v, key)
            self.ccols[key] = v
        return self.ccols[key]

    def recip(self, out, in_, dve=False):
        n = 1
        for d in out.ap.shape[1:]:
            n *= d
        if n <= 16 or dve:
            self.P.add("dve", lambda e: e.reciprocal(out.ap, in_.ap), reads=[in_], writes=[out])
        else:
            self.act(out, in_, AF.Ln)
            self.act(out, out, AF.Exp, scale=-1.0)

    def rmax(self, out, in_):
        self.P.add("dve", lambda e: e.reduce_max(out.ap, in_.ap, AX.X), reads=[in_], writes=[out])

    def memset(self, out, val, eng="dve"):
        self.P.add(eng, lambda e: e.memset(out.ap, val), writes=[out])

    def dma(self, q, out, in_, pairs=None):
        if pairs is None:
            pairs = [(out.ap, in_.ap)]
        n = len(pairs)

        def fn(e):
            return [e.dma_start(out=o, in_=i) for o, i in pairs]

        sembuf = in_.bs[0] if isinstance(out, DV) else out.bs[0]
        self.P.add(q, fn, reads=[in_], writes=[out], dma=n, sembuf=sembuf)


    def build(self):
        nc, P = self.nc, self.P
        es = self.es
        x_d = self.din("x", [SEQ, D])
        ctx_d = self.din("ctx", [CTX, D])
        cvec_d = self.din("cvec", [128, 16])
        pp_d = self.din("pp", [128, DEPTH * NPAR])
        cbf_d = self.din("cbf", [128, 6 * 128])
        cf32_d = self.din("cf32", [128, 4 * 128])
        sel_d = self.din("sel", [8, 8 * 128])
        rope_d = self.din("rope", [128, 2 * SEQ])
        ada_w_d = self.din("ada_w", [DEPTH, D, 6 * D])
        w_in_d = self.din("w_in", [DEPTH, D, IN_COLS])
        w_out_d = self.din("w_out", [DEPTH, D, D])
        ffn_w1_d = self.din("ffn_w1", [1, D, D_FF])
        ffn_w3_d = self.din("ffn_w3", [1, D, D_FF])
        ffn_w2_d = self.din("ffn_w2", [1, D_FF, D])
        router_d = self.din("router_w", [1, D, NEXP])
        moe_w1_d = self.din("moe_w1", [1, NEXP, D, D_FFE])
        moe_w3_d = self.din("moe_w3", [1, NEXP, D, D_FFE])
        moe_w2_d = self.din("moe_w2", [1, NEXP, D_FFE, D])
        y_d = self.dout("y", [SEQ, D])
        self.dbg_outs = {}

        self.ps = [Tn(es.enter_context(nc.psum_tensor(f"ps{i}", [128, 512], F32)), f"ps{i}") for i in range(8)]
        ps = self.ps

        xT = self.sb("xT", [128, 8, NT], F32)
        hT = None
        cbf = self.sb("cbf", [128, 6 * 128], BF16)
        cf32 = self.sb("cf32", [128, 4 * 128], F32)
        sel = self.sb("sel", [8, 8 * 128], BF16) if not SPARSE_MOE else None
        pp = self.sb("pp", [128, DEPTH * NPAR], F32)
        cvec = self.sb("cvec", [128, 16], F32)
        self.xT, self.hT = xT, hT

        q = "pool"
        self.dma("sp", pp.v(0), DV(pp_d[:, :], [Buf("d")]))
        self.dma("sp", cvec.v(0), DV(cvec_d[:, :], [Buf("d")]))
        self.dma("sp", cf32.v(0), DV(cf32_d[:, :], [Buf("d")]))
        self.dma(q, cbf.v(0), DV(cbf_d[:, :], [Buf("d")]))
        if sel is not None:
            self.dma(q, sel.v(0), DV(sel_d[:, :], [Buf("d")]))

        def cb(i):
            return cbf.v(0, slice(None), slice(i * 128, (i + 1) * 128))

        def cf(i):
            return cf32.v(0, slice(None), slice(i * 128, (i + 1) * 128))

        self.ident_bf, self.ones_bf, self.blk64, self.perm, self.maskF, self.maskB = [cb(i) for i in range(6)]
        self.ident_f, self.triF, self.triB, self.ones_f = [cf(i) for i in range(4)]
        self.sel = sel
        self.pp = pp
        self.cvec = cvec

        S = slice
        ALL = slice(None)

        def tile_of_blk(b):
            return b // 4 if b < 16 else 4

        self.tile_of_blk = tile_of_blk

        self.lay = {}
        self.lay_t = {}
        for l_ in range(DEPTH):
            self.lay_t[l_] = (self.sb("mod", [128, 48, 2], F32),
                              self.sb("AB", [128, 2, 8, 2], F32),
                              self.sb("scb", [128, 8, 2], BF16),
                              self.sb("smal", [128, 16], F32))
        with ExitStack() as es0:
            stg = self.sb("xstage", [128, 2, D], F32, es0)
            adaw = self.sb("adaw", [128, 3, 8, 512], BF16, es0)
            junk = self.sb("junk", [128, 2, 64], F32, es0)
            self.prologue(0, ada_w_d, adaw, junk)
            for tb in range(NBLK):
                src = x_d[tb * 128:(tb + 1) * 128, :] if tb < 16 else ctx_d[(tb - 16) * 128:(tb - 15) * 128, :]
                self.dma("sp", stg.v(tb % 2, ALL, tb % 2, ALL), DV(src, [Buf("d")]))
                ti = tile_of_blk(tb)
                for half in range(2):
                    bank = ps[(tb * 2 + half) % 4]
                    for cc in range(4):
                        c = half * 4 + cc
                        self.tr(bank.v(0, ALL, S(cc * 128, (cc + 1) * 128)),
                                stg.v(tb % 2, ALL, tb % 2, S(c * 128, (c + 1) * 128)), self.ident_f)
                    dst = xT.v([(c, ti) for c in range(half * 4, half * 4 + 4)], ALL, S(half * 4, half * 4 + 4),
                               S(tb * 128, (tb + 1) * 128))
                    srcv = V(bank.t[:, :].rearrange("p (a b) -> p a b", a=4), bank.v(0).bs)
                    self.copy(dst, srcv, eng="act" if half == 0 else "dve")
            self.prologue(1, ada_w_d, adaw, junk)
        if self.stage <= -1:
            return self.finish_debug({"xT": (xT, F32, [128, 8, NT])})
        P.barrier()

        for l in range(DEPTH):
            r = self.layer(l, ada_w_d, w_in_d, w_out_d, ffn_w1_d, ffn_w3_d, ffn_w2_d, router_d,
                           moe_w1_d, moe_w3_d, moe_w2_d, rope_d)
            if r is not None:
                return r

        with ExitStack() as eso:
            ost = self.sb("ostage", [128, 2, D], F32, eso)
            outs = []
            for tb in range(16):
                ti = tile_of_blk(tb)
                for half in range(2):
                    bank = ps[(tb * 2 + half) % 4]
                    for cc in range(4):
                        c = half * 4 + cc
                        self.tr(bank.v(0, ALL, S(cc * 128, (cc + 1) * 128)),
                                xT.v((c, ti), ALL, c, S(tb * 128, (tb + 1) * 128)), self.ident_f)
                    self.copy(ost.v((tb % 2, half), ALL, tb % 2, S(half * 512, (half + 1) * 512)), bank.v(0),
                              eng="act" if half == 0 else "dve")
                ov = DV(y_d[tb * 128:(tb + 1) * 128, :], [Buf("yout")])
                self.dma("sp", ov, ost.v([(tb % 2, 0), (tb % 2, 1)], ALL, tb % 2, ALL))
                outs.append(ov)
            P.add("sp", None, reads=outs)
        self.P.emit()
        return None

    def finish_debug(self, tensors):
        self.P.barrier()
        outs = []
        for name, (tn, dt, shape) in tensors.items():
            d = self.dout("dbg_" + name, shape, dt)
            ov = DV(d[tuple(slice(None) for _ in shape)], [Buf("dbg")])
            allb = list(tn.bufs.keys())
            self.dma("sp", ov, tn.v(allb))
            outs.append(ov)
        self.P.add("sp", None, reads=outs)
        self.P.emit()
        return list(tensors.keys())


def _consts():
    p = np.arange(128)
    ident = np.eye(128, dtype=np.float32)
    ones = np.ones((128, 128), np.float32)
    blk64 = (p[:, None] // 64 == p[None, :] // 64).astype(np.float32)
    perm = np.zeros((128, 128), np.float32)
    for j in range(128):
        if j % 64 < 32:
            perm[j + 32, j] = -1.0
        else:
            perm[j - 32, j] = 1.0
    maskF = (p[:, None] <= p[None, :]).astype(np.float32)
    maskB = (p[:, None] >= p[None, :]).astype(np.float32)
    cbf = np.concatenate([ident, ones, blk64, perm, maskF, maskB], axis=1)
    cf32 = np.concatenate([ident, maskF, maskB, ones], axis=1)
    sel = np.zeros((8, 8, 128), np.float32)
    for e in range(8):
        sel[e, e, :] = 1.0
    sel = sel.reshape(8, 8 * 128)
    rows = SEQ // 64
    row = np.repeat(np.arange(rows, dtype=np.float32), 64)
    col = np.tile(np.arange(64, dtype=np.float32), rows)
    inv = (10000.0 ** (-np.arange(16, dtype=np.float32) / 16)).astype(np.float32)
    ang = np.concatenate([row[:, None] * inv, col[:, None] * inv], axis=-1)
    cosT = np.cos(ang).T.astype(np.float32)
    sinT = np.sin(ang).T.astype(np.float32)
    C = np.tile(cosT, (4, 1))
    Sn = np.tile(sinT, (4, 1))
    rope = np.concatenate([C, Sn], axis=1).astype(np.float32)
    iota = np.broadcast_to(np.arange(512, dtype=np.float32)[None, :], (128, 512)).copy()
    return cbf, cf32, sel, rope, iota


def _pack_params(inp):
    pp = np.zeros((DEPTH, 128, NPAR), np.float32)

    def pc(v):
        return np.ascontiguousarray(v.reshape(-1, 128).T)

    for l in range(DEPTH):
        pp[l, :, 0:8] = pc(inp["norm1_g"][l])
        pp[l, :, 8:16] = pc(inp["norm2_g"][l])
        pp[l, :, 16:64] = pc(inp["ada_b"][l])
        pp[l, :, 64] = np.tile(inp["q_norm_g"][l], 2)
        pp[l, :, 65] = np.tile(inp["k_norm_g"][l], 2)
        pp[l, :, 66] = inp["subln_g"][l]
        pp[l, :, 67:71] = pc(inp["mnorm_g"][l])
        cw = inp["conv_w"][l]
        for j in range(5):
            pp[l, :, 71 + j * 8:71 + (j + 1) * 8] = pc(cw[j])
        pp[l, :, 111:119] = pc(inp["conv_b"][l])
        pp[l, :, 119:135] = np.broadcast_to(inp["gate_b"][l].reshape(1, 16), (128, 16))
        pp[l, :, 135:199] = np.broadcast_to(inp["lambda_q1"][l][None, :], (128, 64))
        pp[l, :, 199:263] = np.broadcast_to(inp["lambda_k1"][l][None, :], (128, 64))
        pp[l, :, 263:327] = np.broadcast_to(inp["lambda_q2"][l][None, :], (128, 64))
        pp[l, :, 327:391] = np.broadcast_to(inp["lambda_k2"][l][None, :], (128, 64))
    return np.ascontiguousarray(pp.transpose(1, 0, 2).reshape(128, DEPTH * NPAR))


def _in_maps(inp):
    inp = {k: np.asarray(v, dtype=np.float32) for k, v in inp.items()}
    cbf, cf32, sel, rope, iota = _consts()
    pp = _pack_params(inp)
    shared = {
        "pp": pp, "cbf": cbf, "cf32": cf32, "sel": sel, "rope": rope, "iota": iota,
        "ada_w": inp["ada_w"], "w_in": inp["w_in"], "w_out": inp["w_out"],
        "ffn_w1": inp["ffn_w1"], "ffn_w3": inp["ffn_w3"], "ffn_w2": inp["ffn_w2"],
        "router_w": inp["router_w"], "moe_w1": inp["moe_w1"], "moe_w3": inp["moe_w3"], "moe_w2": inp["moe_w2"],
    }
    if SPARSE_MOE:
        w1, w3, w2 = inp["moe_w1"][0], inp["moe_w3"][0], inp["moe_w2"][0]
        for b in range(14):
            shared[f"m1_{b}"] = np.ascontiguousarray(w1[:, :, b * 256:(b + 1) * 256])
            shared[f"m3_{b}"] = np.ascontiguousarray(w3[:, :, b * 256:(b + 1) * 256])
        for rp in range(7):
            for dh in range(2):
                shared[f"m2_{rp}_{dh}"] = np.ascontiguousarray(w2[:, rp * 512:(rp + 1) * 512, dh * 512:(dh + 1) * 512])
    maps = []
    cc = inp["c_ctx"].reshape(8, 128).T
    for b in range(8):
        m = dict(shared)
        m["x"] = np.ascontiguousarray(inp["x"][b])
        m["ctx"] = np.ascontiguousarray(inp["ctx"][b])
        cv = np.zeros((128, 16), np.float32)
        cv[:, 0:8] = inp["c"][b].reshape(8, 128).T
        cv[:, 8:16] = cc
        m["cvec"] = cv
        maps.append(m)
    return maps


def kernel(**inputs):
    bld = Builder()
    bld.build()
    maps = _in_maps(inputs)
    used = set(bld.dram.keys())
    maps = [{k: v for k, v in m.items() if k in used} for m in maps]
    res = run_bass_kernel_spmd(bld.nc, maps, core_ids=list(range(8)))
    bld.es.close()
    return np.stack([np.asarray(r["y"], dtype=np.float32) for r in res.results], axis=0)


S_ = slice
ALL_ = slice(None)


def _prologue(self, l, ada_w_d, wsl, junk):
    P, ps, pp = self.P, self.ps, self.pp
    S, ALL = S_, ALL_
    lam_init = float(0.8 - 0.6 * np.exp(np.float32(-0.3 * l)))
    pb = l * NPAR

    def ppv(a, b=None):
        if b is None:
            return pp.v(0, ALL, S(pb + a, pb + a + 1))
        return pp.v(0, ALL, S(pb + a, pb + b))

    mod, AB, scb, smal = self.lay_t[l]
    for s in range(2):
        self.act(scb.v(0, ALL, ALL, s), self.cvec_v(s), AF.Silu)
    pm = ps[7]
    for blk in range(12):
        sl = blk % 3
        src = ada_w_d[l, :, blk * 512:(blk + 1) * 512].rearrange("(k p) c -> p k c", p=128)
        self.dma("pool", wsl.v(sl, ALL, sl, ALL, ALL), DV(src, [Buf("d")]))
        for j in range(4):
            col = (blk * 4 + j) * 2
            for k in range(8):
                self.mm(pm.v(0, ALL, S(col, col + 2)), wsl.v(sl, ALL, sl, k, S(j * 128, (j + 1) * 128)),
                        scb.v(0, ALL, k, ALL), start=(k == 0), stop=(k == 7))
    pmv = V(pm.t[:, 0:96].rearrange("p (a b) -> p a b", b=2), pm.v(0).bs)
    for s in range(2):
        self.tt(mod.v(0, ALL, ALL, s), V(pmv.ap[:, :, s], pmv.bs), ppv(16, 64), ALU.add)
    for which in range(2):
        sc_i = 1 if which == 0 else 4
        for s in range(2):
            self.stt(AB.v(0, ALL, which, ALL, s), mod.v(0, ALL, S(sc_i * 8, sc_i * 8 + 8), s), 1.0,
                     ppv(which * 8, which * 8 + 8), ALU.add, ALU.mult)
    self.ts(AB.v(0), AB.v(0), float(np.sqrt(D)), ALU.mult)
    self.tt(junk.v(0, ALL, 0, ALL), ppv(135, 199), ppv(199, 263), ALU.mult)
    self.P.add("dve", lambda e: e.reduce_sum(smal.t[:, 0:1], junk.t[:, 0, :], AX.X),
               reads=[junk.v(0)], writes=[smal.v(0)])
    self.tt(junk.v(1, ALL, 1, ALL), ppv(263, 327), ppv(327, 391), ALU.mult)
    self.P.add("dve", lambda e: e.reduce_sum(smal.t[:, 1:2], junk.t[:, 1, :], AX.X),
               reads=[junk.v(1)], writes=[smal.v(1)])
    self.act(smal.v(2, ALL, S(2, 4)), smal.v([0, 1], ALL, S(0, 2)), AF.Exp)
    self.stt(smal.v(4, ALL, S(4, 5)), smal.v(2, ALL, S(3, 4)), -lam_init, smal.v(2, ALL, S(2, 3)),
             ALU.add, ALU.subtract)
    self.ts(smal.v(5, ALL, S(5, 7)), ppv(64, 66), 8.0, ALU.mult)
    self.ts(smal.v(7, ALL, S(7, 8)), ppv(66), float((1.0 - lam_init) * np.sqrt(128.0)), ALU.mult)
    self.ts(smal.v(8, ALL, S(8, 12)), ppv(67, 71), float(np.sqrt(128.0)), ALU.mult)
    self.lay[l] = (mod, AB, smal)


def _layer(self, l, ada_w_d, w_in_d, w_out_d, ffn_w1_d, ffn_w3_d, ffn_w2_d, router_d,
           moe_w1_d, moe_w3_d, moe_w2_d, rope_d):
    P, ps, xT, pp = self.P, self.ps, self.xT, self.pp
    S, ALL = S_, ALL_
    last = l == DEPTH - 1
    lam_init = float(0.8 - 0.6 * np.exp(np.float32(-0.3 * l)))
    pb = l * NPAR

    def ppv(a, b=None):
        if b is None:
            return pp.v(0, ALL, S(pb + a, pb + a + 1))
        return pp.v(0, ALL, S(pb + a, pb + b))

    tiles = TILES if not last else TILES
    out_tiles = TILES if not last else TILES[:4]

    mod, AB, smal = self.lay[l]

    def modv(i, c, s):
        return mod.v(0, ALL, S(i * 8 + c, i * 8 + c + 1), s)

    def ABv(which, c, s):
        return AB.v(0, ALL, which, S(c, c + 1), s)

    if True:
        with ExitStack() as esm:
            hT = self.hT = self.sb("hT", [128, 8, NT], BF16, esm)
            self.norm_mod(0, TILES, ABv, modv, 0)
            if self.stage == l * 10 + 1:
                return self.finish_debug({"hT": (hT, BF16, [128, 8, NT])})
            P.barrier()
            self.slots = self.sb("wslot", [128, 3, 4096], BF16, esm)
            self.nslots = 3
            self.slot_i = 0
            catA = self.sb("catA", [128, 4, NT], BF16, esm)
            self.attn_heads(l, w_in_d, rope_d, catA, smal, last)
            if self.stage == l * 10 + 2:
                return self.finish_debug({"catA": (catA, BF16, [128, 4, NT])})
            out_tiles = [0, 1, 2, 3] if last else [0, 1, 2, 3, 4]
            self.w_out_half(l, w_out_d, 0, catA, out_tiles, modv)
            P.barrier()
            gsc = self.gates(l, w_in_d, esm)
            if self.stage == l * 10 + 3:
                return self.finish_debug({"ga": (gsc[0], F32, [128, NBLK, 8]), "gc": (gsc[1], F32, [128, NBLK, 8]),
                                          "gib": (gsc[2], F32, [128, NBLK, 8]), "gd": (gsc[3], F32, [128, NBLK, 8])})
            self.mlstm_heads(l, w_in_d, catA, smal, last, gsc)
            if self.stage == l * 10 + 4:
                return self.finish_debug({"catM": (catA, BF16, [128, 4, NT])})
            self.w_out_half(l, w_out_d, 1, catA, out_tiles, modv)
            if self.stage == l * 10 + 5:
                return self.finish_debug({"xT": (xT, F32, [128, 8, NT])})
            P.barrier()
        if l % 2 == 0:
            with ExitStack() as esh:
                hT = self.hT = self.sb("hT", [128, 8, NT], BF16, esh)
                self.norm_mod(1, TILES, ABv, modv, 3)
                if self.stage == l * 10 + 6:
                    return self.finish_debug({"hT": (hT, BF16, [128, 8, NT])})
                r = self.ffn_dense(l, ffn_w1_d, ffn_w3_d, ffn_w2_d, modv)
                if r is not None:
                    return r
        else:
            if SPARSE_MOE:
                r = self.moe_sparse(l, router_d, ABv, modv)
            else:
                with ExitStack() as esh:
                    self.hT = self.sb("hT", [128, 8, NT], BF16, esh)
                    r = self.moe(l, router_d, moe_w1_d, moe_w3_d, moe_w2_d, ABv, modv)
            if r is not None:
                return r
        if self.stage == l * 10 + 7:
            return self.finish_debug({"xT": (xT, F32, [128, 8, NT])})
        P.barrier()
    return None


def _cvec_v(self, s):
    return self.cvec.v(0, ALL_, S_(s * 8, s * 8 + 8))


def _norm_mod(self, which, tiles, ABv, modv, shift_i, router=None, dst=None, after_tile=None):
    ps, xT, hT = self.ps, self.xT, self.hT
    S, ALL = S_, ALL_
    with ExitStack() as esn:
        sq = self.sb("nsq", [128, 2, 512], BF16, esn)
        rstd = self.sb("nrstd", [128, 2, 512], F32, esn)
        tmp = self.sb("ntmp", [128, 2, 512], F32, esn)
        hf = self.sb("nhf", [128, 2, 512], F32, esn) if router is not None else None
        for ti, (t0, n) in enumerate(TILES):
            if (t0, n) not in tiles:
                continue
            s = 0 if ti < 4 else 1
            bank = ps[ti % 2]
            for c in range(8):
                self.act(sq.v(c % 2, ALL, c % 2, S(0, n)), xT.v((c, ti), ALL, c, S(t0, t0 + n)), AF.Square)
                self.mm(bank.v(0, ALL, S(0, n)), self.ones_bf, sq.v(c % 2, ALL, c % 2, S(0, n)),
                        start=(c == 0), stop=(c == 7))
            r = rstd.v(ti % 2, ALL, ti % 2, S(0, n))
            self.rsqrt(r, bank.v(0, ALL, S(0, n)), float(D * EPS))
            for c in range(8):
                t = tmp.v(c % 2, ALL, c % 2, S(0, n))
                self.tt(t, xT.v((c, ti), ALL, c, S(t0, t0 + n)), r, ALU.mult)
                if router is None:
                    self.act(hT.v((c, ti), ALL, c, S(t0, t0 + n)), t, AF.Identity,
                             bias=modv(shift_i, c, s), scale=ABv(which, c, s))
                else:
                    wr, pR = router
                    hv = hf.v(c % 2, ALL, c % 2, S(0, n))
                    self.act(hv, t, AF.Identity, bias=modv(shift_i, c, s), scale=ABv(which, c, s))
                    self.copy(dst(c, ti, n) if dst is not None else hT.v((c, ti), ALL, c, S(t0, t0 + n)), hv)
                    for bi in range(n // 128):
                        col = (ti * 4 + bi) * 8
                        self.mm(pR.v(0, ALL, S(col, col + 8)), hf.v(c % 2, ALL, c % 2, S(bi * 128, (bi + 1) * 128)),
                                wr.v(0, ALL, c, ALL), start=(c == 0 and bi == 0), stop=(c == 7), skip=True)
            if after_tile is not None:
                after_tile(ti)
    self.P.barrier()


Builder.layer = _layer
Builder.prologue = _prologue
Builder.cvec_v = _cvec_v
Builder.norm_mod = _norm_mod


def _slot(self):
    i = self.slot_i % self.nslots
    self.slot_i += 1
    return i


def _attn_heads(self, l, w_in_d, rope_d, catA, smal, last):
    P, ps, hT = self.P, self.ps, self.hT
    S, ALL = S_, ALL_
    slots = self.slots
    tob = self.tile_of_blk
    neglam = smal.v(4, ALL, S(4, 5))
    gs = smal.v(7, ALL, S(7, 8))
    with ExitStack() as esw:
        rope = self.sb("rope", [128, 2 * SEQ], BF16, esw)
        qT = self.sb("qT", [128, NT], BF16, esw)
        kT = self.sb("kT", [128, NT], BF16, esw)
        vtok = self.sb("vtok", [128, NBLK, 128], BF16, esw)
        pT = self.sb("pT", [128, 4, 512], BF16, esw)
        sqb = self.sb("sqb", [128, 2, 512], BF16, esw)
        qn = self.sb("qn", [128, 2, 512], BF16, esw)
        tf = self.sb("tf", [128, 6, 512], F32, esw)
        self.dma("pool", rope.v(0), DV(rope_d[:, :], [Buf("d")]))
        for h in range(4):
            si = self.slot()
            wv = slots.t[:, si, 0:8 * 384].rearrange("p (k c) -> p k c", c=384)
            wb = slots.v(si).bs
            pairs = []
            for j, base in enumerate((0, 512, 1024)):
                col = base + h * 128
                pairs.append((wv[:, :, j * 128:(j + 1) * 128],
                              w_in_d[l, :, col:col + 128].rearrange("(k p) c -> p k c", p=128)))
            self.dma("pool", V(wv, wb), DV(pairs[0][1], [Buf("d")]), pairs=pairs)
            groups = []
            for which, dest in ((0, qT), (1, kT)):
                for ti, (t0, n) in enumerate(TILES):
                    if which == 0 and last and ti == 4:
                        continue
                    groups.append((which, dest, ti, t0, n))
            pa = [ps[0], ps[1], ps[2]]
            pbk = [ps[3], ps[4]]
            pck = [ps[5], ps[6]]

            def stageA(gi):
                which, dest, ti, t0, n = groups[gi]
                bA = pa[gi % 3]
                for k in range(8):
                    self.mm(bA.v(0, ALL, S(0, n)), V(wv[:, k, which * 128:(which + 1) * 128], wb),
                            hT.v((k, ti), ALL, k, S(t0, t0 + n)), start=(k == 0), stop=(k == 7))

            stageA(0)
            if len(groups) > 1:
                stageA(1)
            for gi, (which, dest, ti, t0, n) in enumerate(groups):
                g8 = smal.v(5 + which, ALL, S(5 + which, 6 + which))
                par = gi % 2
                bA, bB, bC = pa[gi % 3], pbk[par], pck[par]
                sq = sqb.v(par, ALL, par, S(0, n))
                self.act(sq, bA.v(0, ALL, S(0, n)), AF.Square)
                self.mm(bB.v(0, ALL, S(0, n)), self.blk64, sq)
                if gi + 2 < len(groups):
                    stageA(gi + 2)
                rstd = tf.v(par, ALL, par, S(0, n))
                self.rsqrt(rstd, bB.v(0, ALL, S(0, n)), float(64 * EPS))
                dv = dest.v(ti, ALL, S(t0, t0 + n))
                if ti < 4:
                    qv = qn.v(par, ALL, par, S(0, n))
                    self.stt(qv, bA.v(0, ALL, S(0, n)), g8, rstd, ALU.mult, ALU.mult)
                    self.mm(bC.v(0, ALL, S(0, n)), self.perm, qv)
                    t1 = tf.v(2 + par, ALL, 2 + par, S(0, n))
                    t2 = tf.v(4 + par, ALL, 4 + par, S(0, n))
                    self.tt(t1, qv, rope.v(0, ALL, S(t0, t0 + n)), ALU.mult)
                    self.tt(t2, bC.v(0, ALL, S(0, n)), rope.v(0, ALL, S(SEQ + t0, SEQ + t0 + n)), ALU.mult)
                    self.tt(dv, t1, t2, ALU.add)
                else:
                    self.stt(dv, bA.v(0, ALL, S(0, n)), g8, rstd, ALU.mult, ALU.mult)
            for g in range(5):
                blks = list(range(g * 4, min(g * 4 + 4, NBLK)))
                bank = ps[7 - g % 2]
                for bi, blk in enumerate(blks):
                    for k in range(8):
                        self.mm(bank.v(0, ALL, S(bi * 128, (bi + 1) * 128)),
                                hT.v((k, tob(blk)), ALL, k, S(blk * 128, (blk + 1) * 128)),
                                V(wv[:, k, 256:384], wb), start=(k == 0), stop=(k == 7))
                nb = len(blks)
                srcv = V(bank.t[:, 0:nb * 128].rearrange("p (a b) -> p a b", b=128), bank.v(0).bs)
                self.copy(vtok.v(g, ALL, S(blks[0], blks[0] + nb), ALL), srcv, eng="act")
            qtiles = [(ti, list(range(NBLK))) for ti in range(4)]
            if not last:
                qtiles.append((4, [16, 17]))
            pending = []
            for ti, kblks in qtiles:
                t0, n = TILES[ti]
                psS = [[ps[0], ps[1]], [ps[2], ps[3]]]
                psO = [ps[4], ps[5]]
                psZ = [ps[6], ps[7]]
                nk = len(kblks)

                def Sstep(i):
                    kb = kblks[i]
                    for c in range(2):
                        self.mm(psS[c][i % 2].v(0, ALL, S(0, n)),
                                kT.v(tob(kb), S(c * 64, (c + 1) * 64), S(kb * 128, (kb + 1) * 128)),
                                qT.v(ti, S(c * 64, (c + 1) * 64), S(t0, t0 + n)))

                Sstep(0)
                for i in range(nk):
                    if i + 1 < nk:
                        Sstep(i + 1)
                    kb = kblks[i]
                    for c in range(2):
                        pv = pT.v((c, i % 2), ALL, c * 2 + i % 2, S(0, n))
                        self.act(pv, psS[c][i % 2].v(0, ALL, S(0, n)), AF.Exp, scale=0.125)
                    for c in range(2):
                        pv = pT.v((c, i % 2), ALL, c * 2 + i % 2, S(0, n))
                        self.mm(psO[c].v(0, ALL, S(0, n)), vtok.v(kb // 4, ALL, kb, ALL), pv,
                                start=(i == 0), stop=(i == nk - 1))
                        self.mm(psZ[c].v(0, ALL, S(0, n)), self.ones_bf, pv,
                                start=(i == 0), stop=(i == nk - 1))
                    if pending and i in (1, 9):
                        pending.pop(0)()
                while pending:
                    pending.pop(0)()
                zc1 = tf.v(0, ALL, 0, S(0, n))
                zc2 = tf.v(1, ALL, 1, S(0, n))
                o1 = tf.v(2, ALL, 2, S(0, n))
                o2 = tf.v(3, ALL, 3, S(0, n))
                rs = tf.v(4, ALL, 4, S(0, n))
                self.copy(zc1, psZ[0].v(0, ALL, S(0, n)), eng="dve")
                self.copy(zc2, psZ[1].v(0, ALL, S(0, n)), eng="dve")
                self.copy(o1, psO[0].v(0, ALL, S(0, n)), eng="act")
                self.copy(o2, psO[1].v(0, ALL, S(0, n)), eng="act")

                def fin1(zc1=zc1, zc2=zc2, o1=o1, o2=o2, n=n):
                    self.recip(zc1, zc1)
                    self.recip(zc2, zc2)
                    self.tt(o1, o1, zc1, ALU.mult)
                    self.tt(o2, o2, zc2, ALU.mult)
                    self.stt(o1, o2, neglam, o1, ALU.mult, ALU.add)
                    self.act(sqb.v(0, ALL, 0, S(0, n)), o1, AF.Square)

                def fin2(o1=o1, rs=rs, n=n, h=h, ti=ti, t0=t0):
                    sq = sqb.v(0, ALL, 0, S(0, n))
                    self.mm(ps[1].v(0, ALL, S(0, n)), self.ones_bf, sq)
                    self.rsqrt(rs, ps[1].v(0, ALL, S(0, n)), float(128 * EPS))
                    self.stt(catA.v((h, ti), ALL, h, S(t0, t0 + n)), o1, gs, rs, ALU.mult, ALU.mult)

                pending.extend([fin1, fin2])
            while pending:
                pending.pop(0)()


Builder.slot = _slot
Builder.attn_heads = _attn_heads


def _gates(self, l, w_in_d, esm):
    ps, hT, pp = self.ps, self.hT, self.pp
    S, ALL = S_, ALL_
    tob = self.tile_of_blk
    pb = l * NPAR
    lns = float(-0.5 * np.log(128.0))
    G = self.sb("G", [128, NBLK, 16], F32, esm)
    sp = self.sb("sp", [128, NBLK, 8], F32, esm)
    Bs = self.sb("Bs", [128, NBLK, 8], F32, esm)
    Bt = self.sb("Bt", [128, NBLK, 8], F32, esm)
    E1 = self.sb("E1", [128, NBLK, 8], F32, esm)
    ga = self.sb("ga", [128, NBLK, 8], F32, esm)
    gc = self.sb("gc", [128, NBLK, 8], F32, esm)
    gib = self.sb("gib", [128, NBLK, 8], F32, esm)
    gd = self.sb("gd", [128, NBLK, 8], F32, esm)
    si = self.slot()
    wv = self.slots.t[:, si, 0:128].rearrange("p (k c) -> p k c", c=16)
    wb = self.slots.v(si).bs
    self.dma("pool", V(wv, wb), DV(w_in_d[l, :, 3584:3600].rearrange("(k p) c -> p k c", p=128), [Buf("d")]))
    pG = ps[0]
    for blk in range(NBLK):
        for k in range(8):
            self.mm(pG.v(0, ALL, S(blk * 16, blk * 16 + 16)), hT.v((k, tob(blk)), ALL, k, S(blk * 128, (blk + 1) * 128)),
                    V(wv[:, k, :], wb), start=(k == 0), stop=(k == 7))
    for blk in range(NBLK):
        self.tt(G.v(0, ALL, blk, ALL), pG.v(0, ALL, S(blk * 16, blk * 16 + 16)), pp.v(0, ALL, S(pb + 119, pb + 135)),
                ALU.add)
    for d in range(2):
        self.act(sp.v(0, ALL, ALL, S(d * 4, d * 4 + 4)), G.v(0, ALL, ALL, S(d * 8 + 4, d * 8 + 8)), AF.Exp, scale=-1.0)
    self.act(sp.v(0), sp.v(0), AF.Ln, bias=self.constcol(1.0))
    pB, pT_ = ps[1], ps[2]
    for blk in range(NBLK):
        self.mm(pB.v(0, ALL, S(blk * 8, blk * 8 + 4)), self.triF, sp.v(0, ALL, blk, S(0, 4)))
        self.mm(pB.v(0, ALL, S(blk * 8 + 4, blk * 8 + 8)), self.triB, sp.v(0, ALL, blk, S(4, 8)))
        self.mm(pT_.v(0, ALL, S(blk * 8, blk * 8 + 8)), self.ones_f, sp.v(0, ALL, blk, ALL))
    bsv = V(Bs.t[:, :, :].rearrange("p a b -> p (a b)"), Bs.v(0).bs)
    btv = V(Bt.t[:, :, :].rearrange("p a b -> p (a b)"), Bt.v(0).bs)
    self.copy(bsv, pB.v(0, ALL, S(0, NBLK * 8)))
    self.copy(btv, pT_.v(0, ALL, S(0, NBLK * 8)))
    for d in range(2):
        self.tt(E1.v(0, ALL, ALL, S(d * 4, d * 4 + 4)), G.v(0, ALL, ALL, S(d * 8, d * 8 + 4)),
                Bs.v(0, ALL, ALL, S(d * 4, d * 4 + 4)), ALU.add)
    self.act(ga.v(0), E1.v(0), AF.Exp, bias=self.constcol(lns))
    self.tt(E1.v(0), E1.v(0), Bt.v(0), ALU.subtract)
    self.act(gc.v(0), E1.v(0), AF.Exp, bias=self.constcol(lns))
    self.act(gib.v(0), Bs.v(0), AF.Exp)
    self.act(gd.v(0), Bt.v(0), AF.Exp, scale=-1.0)
    return ga, gc, gib, gd


def _mlstm_heads(self, l, w_in_d, catM, smal, last, gsc):
    P, ps, hT, pp = self.P, self.ps, self.hT, self.pp
    S, ALL = S_, ALL_
    slots = self.slots
    tob = self.tile_of_blk
    pb = l * NPAR
    ga, gc, gib, gd = gsc
    with ExitStack() as esw:
        rawb = self.sb("rawb", [128, NT + 8], BF16, esw)
        qT = self.sb("mqT", [128, NT], BF16, esw)
        kT = self.sb("mkT", [128, NT], BF16, esw)
        sigT = self.sb("sigT", [128, NT], BF16, esw)
        vaug = self.sb("vaug", [128, NBLK, 130], BF16, esw)
        hsum = self.sb("hsum", [128, NBLK, 128], F32, esw)
        dg = self.sb("dg", [128, 10, 128], BF16, esw)
        sTm = self.sb("sTm", [128, 4, 128], BF16, esw)
        khat = self.sb("khat", [128, 4, 128], BF16, esw)
        Cf = self.sb("Cf", [128, 2, 130], F32, esw)
        Cb = self.sb("Cb", [128, 2, 130], BF16, esw)
        rr = self.sb("rr", [128, 2, 4], F32, esw)
        hn = self.sb("hn", [128, 2, 128], BF16, esw)
        jk = self.sb("jk", [128, 2, 128], F32, esw)
        ssq = self.sb("ssq", [128, 2, 2], F32, esw)
        self.memset(rawb.v(0), 0.0)
        self.memset(vaug.v("ones", ALL, ALL, S(128, 130)), 1.0)
        psK = [V(ps[6].t[:, :].bitcast(BF16), ps[6].v(0).bs), V(ps[7].t[:, :].bitcast(BF16), ps[7].v(0).bs)]
        for h in range(4):
            si = self.slot()
            wv = slots.t[:, si, 0:8 * 512].rearrange("p (k c) -> p k c", c=512)
            wb = slots.v(si).bs
            pairs = []
            for j, base in enumerate((1536, 2048, 2560, 3072)):
                col = base + h * 128
                pairs.append((wv[:, :, j * 128:(j + 1) * 128],
                              w_in_d[l, :, col:col + 128].rearrange("(k p) c -> p k c", p=128)))
            self.dma("pool", V(wv, wb), DV(pairs[0][1], [Buf("d")]), pairs=pairs)
            for w in range(2):
                c = w * 4 + h
                for j in range(5):
                    col = pb + 71 + j * 8 + c
                    self.ts(dg.v(w, ALL, w * 5 + j, ALL), self.ident_bf, pp.v(0, ALL, S(col, col + 1)), ALU.mult)
            for w, dest in ((0, qT), (1, kT)):
                c = w * 4 + h
                for ti, (t0, n) in enumerate(TILES):
                    bank = ps[ti % 2]
                    for k in range(8):
                        self.mm(bank.v(0, ALL, S(0, n)), V(wv[:, k, w * 128:(w + 1) * 128], wb),
                                hT.v((k, ti), ALL, k, S(t0, t0 + n)), start=(k == 0), stop=(k == 7))
                    off = t0 + 2 if ti < 4 else t0 + 6
                    self.copy(rawb.v(0, ALL, S(off, off + n)), bank.v(0, ALL, S(0, n)), eng="act")
                for ti, (t0, n) in enumerate(TILES):
                    bank = ps[2 + ti % 2]
                    off = t0 if ti < 4 else t0 + 4
                    for j in range(5):
                        self.mm(bank.v(0, ALL, S(0, n)), dg.v(w, ALL, w * 5 + j, ALL),
                                rawb.v(0, ALL, S(off + j, off + j + n)), start=(j == 0), stop=(j == 4))
                    self.act(dest.v(0, ALL, S(t0, t0 + n)), bank.v(0, ALL, S(0, n)), AF.Silu,
                             bias=pp.v(0, ALL, S(pb + 111 + c, pb + 112 + c)))
            for g in range(5):
                blks = list(range(g * 4, min(g * 4 + 4, NBLK)))
                bank = ps[4 + g % 2]
                for bi, blk in enumerate(blks):
                    for k in range(8):
                        self.mm(bank.v(0, ALL, S(bi * 128, (bi + 1) * 128)),
                                hT.v((k, tob(blk)), ALL, k, S(blk * 128, (blk + 1) * 128)),
                                V(wv[:, k, 256:384], wb), start=(k == 0), stop=(k == 7))
                nb = len(blks)
                srcv = V(bank.t[:, 0:nb * 128].rearrange("p (a b) -> p a b", b=128), bank.v(0).bs)
                self.copy(vaug.v(0, ALL, S(blks[0], blks[0] + nb), S(0, 128)), srcv, eng="act")
            for ti, (t0, n) in enumerate(TILES):
                if last and ti == 4:
                    continue
                bank = ps[ti % 2]
                for k in range(8):
                    self.mm(bank.v(0, ALL, S(0, n)), V(wv[:, k, 384:512], wb),
                            hT.v((k, ti), ALL, k, S(t0, t0 + n)), start=(k == 0), stop=(k == 7))
                self.act(sigT.v(0, ALL, S(t0, t0 + n)), bank.v(0, ALL, S(0, n)), AF.Sigmoid)
            order = [[16, 17] + list(range(16)), [17, 16] + list(range(15, -1, -1))]
            masks = [self.maskF, self.maskB]
            visited = set()
            npost = 0
            mg = smal.v(8, ALL, S(8 + h, 9 + h))
            def need_out_of(blk):
                return not (last and blk >= 16)

            def pre(i, d):
                blk = order[d][i]
                col = d * 4 + h
                par = i % 2
                bsl = S(blk * 128, (blk + 1) * 128)
                kv = kT.v(0, ALL, bsl)
                qv = qT.v(0, ALL, bsl)
                if need_out_of(blk):
                    pS = ps[d]
                    self.mm(pS.v(0, ALL, S(0, 128)), kv, qv)
                    sv = sTm.v((d, par), ALL, d * 2 + par, ALL)
                    self.stt(sv, pS.v(0, ALL, S(0, 128)), ga.v(0, ALL, blk, S(col, col + 1)), masks[d],
                             ALU.mult, ALU.mult)
                if i < NBLK - 1:
                    pk = V(psK[d].ap[:, 0:128], psK[d].bs)
                    self.tr(pk, kv, self.ident_bf)
                    kh = khat.v((d, par), ALL, d * 2 + par, ALL)
                    self.act(kh, pk, AF.Copy, scale=gc.v(0, ALL, blk, S(col, col + 1)))

            for d in range(2):
                pre(0, d)
            for i in range(NBLK):
                if i + 1 < NBLK:
                    for d in range(2):
                        pre(i + 1, d)
                for d in range(2):
                    blk = order[d][i]
                    col = d * 4 + h
                    par = i % 2
                    first = i == 0
                    bsl = S(blk * 128, (blk + 1) * 128)
                    pH, pC = ps[2 + d], ps[4 + d]
                    qv = qT.v(0, ALL, bsl)
                    need_out = need_out_of(blk)
                    if need_out:
                        sv = sTm.v((d, par), ALL, d * 2 + par, ALL)
                        self.mm(pH.v(0, ALL, S(0, 129)), sv, vaug.v([0, "ones"], ALL, blk, S(0, 129)),
                                start=True, stop=first)
                        if not first:
                            self.mm(pH.v(0, ALL, S(0, 129)), qv, Cb.v(d, ALL, d, S(0, 129)), start=False, stop=True)
                    if i < NBLK - 1:
                        kh = khat.v((d, par), ALL, d * 2 + par, ALL)
                        self.mm(pC.v(0, ALL, S(0, 129)), kh, vaug.v([0, "ones"], ALL, blk, S(0, 129)))
                        cfv = Cf.v(d, ALL, d, S(0, 129))
                        if first:
                            self.copy(cfv, pC.v(0, ALL, S(0, 129)))
                        else:
                            self.stt(cfv, cfv, gd.v(0, ALL, blk, S(col, col + 1)), pC.v(0, ALL, S(0, 129)),
                                     ALU.mult, ALU.add)
                        self.copy(Cb.v(d, ALL, d, S(0, 129)), cfv, eng="act")
                    if need_out:
                        r0 = rr.v(d, ALL, d, S(0, 1))
                        r1 = rr.v(d, ALL, d, S(1, 2))
                        self.act(r0, pH.v(0, ALL, S(128, 129)), AF.Abs)
                        self.ts(r0, r0, gib.v(0, ALL, blk, S(col, col + 1)), ALU.max)
                        self.recip(r1, r0)
                        hv = hsum.v(blk, ALL, blk, ALL)
                        if blk not in visited:
                            self.act(hv, pH.v(0, ALL, S(0, 128)), AF.Copy, scale=r1)
                        else:
                            self.stt(hv, pH.v(0, ALL, S(0, 128)), r1, hv, ALU.mult, ALU.add)
                    if blk in visited and need_out:
                        npost += 1
                        pq = npost % 2
                        hv = hsum.v(blk, ALL, blk, ALL)
                        jv = jk.v(pq, ALL, pq, ALL)
                        s0 = ssq.v(pq, ALL, pq, S(0, 1))
                        self.act(jv, hv, AF.Square)
                        self.P.add("dve", lambda e, a=s0.ap, b=jv.ap: e.reduce_sum(a, b, AX.X), reads=[jv], writes=[s0])
                        self.rsqrt(s0, s0, float(128 * EPS))
                        hnv = hn.v(pq, ALL, pq, ALL)
                        self.ts(hnv, hv, s0, ALU.mult)
                        pk = V(psK[pq].ap[:, 128:256], psK[pq].bs)
                        self.tr(pk, hnv, self.ident_bf)
                        self.stt(catM.v((h, tob(blk)), ALL, h, bsl), pk, mg, sigT.v(0, ALL, bsl), ALU.mult, ALU.mult)
                    visited.add(blk)


Builder.gates = _gates
Builder.mlstm_heads = _mlstm_heads


def _w_out_half(self, l, w_out_d, half, cat, out_tiles, modv):
    ps, xT = self.ps, self.xT
    S, ALL = S_, ALL_
    si = self.slot()
    wv = self.slots.t[:, si, 0:4096].rearrange("p (k c) -> p k c", c=1024)
    wb = self.slots.v(si).bs
    self.dma("pool", V(wv, wb), DV(w_out_d[l, half * 512:(half + 1) * 512, :].rearrange("(k p) c -> p k c", p=128),
                                   [Buf("d")]))
    cnt = 0
    for m in range(8):
        for ti in out_tiles:
            t0, n = TILES[ti]
            s = 0 if ti < 4 else 1
            bank = ps[cnt % 4]
            cnt += 1
            for k in range(4):
                self.mm(bank.v(0, ALL, S(0, n)), V(wv[:, k, m * 128:(m + 1) * 128], wb),
                        cat.v((k, ti), ALL, k, S(t0, t0 + n)), start=(k == 0), stop=(k == 3))
            xv = xT.v((m, ti), ALL, m, S(t0, t0 + n))
            self.stt(xv, bank.v(0, ALL, S(0, n)), modv(2, m, s), xv, ALU.mult, ALU.add)


Builder.w_out_half = _w_out_half


def _ffn_expert(self, w1_fn, w3_fn, w2_fn, dff, halves, y, tf, gate_v, cb_fn=None, tmpx=None):
    ps, xT, hT = self.ps, self.xT, self.hT
    S, ALL = S_, ALL_
    slots = self.slots
    nch = dff // 128
    for hi, half in enumerate(halves):
        toffs = {}
        o = 0
        for ti in half:
            toffs[ti] = o
            o += TILES[ti][1]
        cb = cb_fn(hi, half, toffs) if cb_fn is not None else None
        for c0 in range(0, nch, 8):
            c1 = min(c0 + 8, nch)
            if getattr(self, "dbg_maxg", None) is not None and (c0 // 8 >= self.dbg_maxg or hi >= 1):
                continue
            for b0 in range(c0, c1, 4):
                b1 = min(b0 + 4, c1)
                ncol = (b1 - b0) * 128
                si = self.slot()
                wv = slots.t[:, si, 0:8192].rearrange("p (w k c) -> p w k c", w=2, k=8)
                wb = slots.v(si).bs
                pairs = [(wv[:, 0, :, 0:ncol], w1_fn(b0 * 128, ncol)), (wv[:, 1, :, 0:ncol], w3_fn(b0 * 128, ncol))]
                self.dma("pool", V(wv, wb), DV(pairs[0][1], [Buf("d")]), pairs=pairs)
                for j in range(b0, b1):
                    jj = j - b0
                    for ti in half:
                        t0, n = TILES[ti]
                        self.cntA += 1
                        par = self.cntA % 2
                        bU, bV = ps[par], ps[2 + par]
                        for k in range(8):
                            self.mm(bU.v(0, ALL, S(0, n)), V(wv[:, 0, k, jj * 128:(jj + 1) * 128], wb),
                                    hT.v((k, ti), ALL, k, S(t0, t0 + n)), start=(k == 0), stop=(k == 7))
                        for k in range(8):
                            self.mm(bV.v(0, ALL, S(0, n)), V(wv[:, 1, k, jj * 128:(jj + 1) * 128], wb),
                                    hT.v((k, ti), ALL, k, S(t0, t0 + n)), start=(k == 0), stop=(k == 7))
                        sv = tf.v(par, ALL, par, S(0, n))
                        self.act(sv, bU.v(0, ALL, S(0, n)), AF.Silu)
                        self.tt(y.v((j - c0, toffs[ti]), ALL, j - c0, S(toffs[ti], toffs[ti] + n)), sv,
                                bV.v(0, ALL, S(0, n)), ALU.mult)
            si = self.slot()
            ng = c1 - c0
            wv = slots.t[:, si, 0:ng * 1024].rearrange("p (k c) -> p k c", c=1024)
            wb = slots.v(si).bs
            self.dma("pool", V(wv, wb), DV(w2_fn(c0 * 128, ng * 128), [Buf("d")]))
            for m in range(8):
                for ti in half:
                    t0, n = TILES[ti]
                    s = 0 if ti < 4 else 1
                    self.cntB += 1
                    bO = ps[4 + self.cntB % 2]
                    for jj in range(ng):
                        self.mm(bO.v(0, ALL, S(0, n)), V(wv[:, jj, m * 128:(m + 1) * 128], wb),
                                y.v((jj, toffs[ti]), ALL, jj, S(toffs[ti], toffs[ti] + n)), start=(jj == 0), stop=(jj == ng - 1))
                    xv = xT.v((m, ti), ALL, m, S(t0, t0 + n))
                    if cb is None:
                        self.stt(xv, bO.v(0, ALL, S(0, n)), gate_v(m, s), xv, ALU.mult, ALU.add)
                    else:
                        tv = tmpx.v(self.cntB % 2, ALL, self.cntB % 2, S(0, n))
                        self.stt(tv, bO.v(0, ALL, S(0, n)), gate_v(m, s), cb.v(toffs[ti], ALL, S(toffs[ti], toffs[ti] + n)),
                                 ALU.mult, ALU.mult)
                        self.tt(xv, xv, tv, ALU.add)


def _ffn_dense(self, l, w1_d, w3_d, w2_d, modv):
    S, ALL = S_, ALL_
    j = l // 2
    with ExitStack() as esf:
        self.slots = self.sb("fslot", [128, 3, 8192], BF16, esf)
        self.nslots = 3
        self.slot_i = 0
        y = self.sb("y", [128, 8, 1280], BF16, esf)
        tf = self.sb("ftf", [128, 2, 512], F32, esf)
        self.cntA = 0
        self.cntB = 0

        def w1_fn(c0, nc_):
            return w1_d[j, :, c0:c0 + nc_].rearrange("(k p) c -> p k c", p=128)

        def w3_fn(c0, nc_):
            return w3_d[j, :, c0:c0 + nc_].rearrange("(k p) c -> p k c", p=128)

        def w2_fn(r0, nr):
            return w2_d[j, r0:r0 + nr, :].rearrange("(k p) c -> p k c", p=128)

        if self.stage == l * 10 + 8:
            self.dbg_maxg = 1
        self.ffn_expert(w1_fn, w3_fn, w2_fn, D_FF, [[0, 1], [2, 3, 4]], y, tf, lambda m, s: modv(5, m, s))
        if self.stage == l * 10 + 8:
            return self.finish_debug({"y": (y, BF16, [128, 8, 1280]), "tf": (tf, F32, [128, 2, 512]),
                                      "xT": (self.xT, F32, [128, 8, NT]), "slots": (self.slots, BF16, [128, 3, 8192])})
        return None


Builder.ffn_expert = _ffn_expert
Builder.ffn_dense = _ffn_dense


def _moe(self, l, router_d, w1_d, w3_d, w2_d, ABv, modv):
    P, ps, hT = self.P, self.ps, self.hT
    S, ALL = S_, ALL_
    j = l // 2
    LT = TILES[:4]
    with ExitStack() as esf:
        wr = self.sb("wr", [128, 8, 8], F32, esf)
        combT = self.sb("combT", [8, SEQ], BF16, esf)
        self.dma("sp", wr.v(0), DV(router_d[j, :, :].rearrange("(k p) e -> p k e", p=128), [Buf("d")]))
        pR = ps[7]
        self.norm_mod(1, LT, ABv, modv, 3, router=(wr, pR))
        if self.stage == l * 10 + 6:
            lgd = self.sb("lgd", [128, 128], F32, esf)
            self.copy(lgd.v(0), pR.v(0, ALL, S(0, 128)))
            return self.finish_debug({"hT": (hT, BF16, [128, 8, NT]), "lg": (lgd, F32, [128, 128])})
        with ExitStack() as est:
            lg = self.sb("lg", [128, 16, 8], F32, est)
            l2 = self.sb("l2", [128, 16, 8], F32, est)
            mk1 = self.sb("mk1", [128, 16, 8], F32, est)
            mk2 = self.sb("mk2", [128, 16, 8], F32, est)
            m1 = self.sb("m1", [128, 16], F32, est)
            m2 = self.sb("m2", [128, 16], F32, est)
            w1 = self.sb("w1", [128, 16], F32, est)
            w2 = self.sb("w2", [128, 16], F32, est)
            comb = self.sb("comb", [128, 16, 8], BF16, est)

            def bc(tn):
                return V(tn.t[:, :].unsqueeze(2).to_broadcast([128, 16, 8]), tn.v(0).bs)

            lgf = V(lg.t[:, :, :].rearrange("p a b -> p (a b)"), lg.v(0).bs)
            self.copy(lgf, pR.v(0, ALL, S(0, 128)))
            self.rmax(m1.v(0), lg.v(0))
            self.tt(mk1.v(0), lg.v(0), bc(m1), ALU.is_equal)
            self.stt(l2.v(0), mk1.v(0), -1e30, lg.v(0), ALU.mult, ALU.add)
            self.rmax(m2.v(0), l2.v(0))
            self.tt(mk2.v(0), l2.v(0), bc(m2), ALU.is_equal)
            self.tt(w2.v(0), m2.v(0), m1.v(0), ALU.subtract)
            self.act(w2.v(0), w2.v(0), AF.Sigmoid)
            self.ts(w1.v(0), w2.v(0), -1.0, ALU.mult, 1.0, ALU.add)
            self.tt(mk1.v(0), mk1.v(0), bc(w1), ALU.mult)
            self.tt(mk2.v(0), mk2.v(0), bc(w2), ALU.mult)
            self.tt(comb.v(0), mk1.v(0), mk2.v(0), ALU.add)
            for g in range(2):
                bank = ps[g]
                pk = bank.t[:, :].bitcast(BF16)
                for bi in range(8):
                    blk = g * 8 + bi
                    self.tr(V(pk[0:8, bi * 128:(bi + 1) * 128], bank.v(0).bs), comb.v(0, ALL, blk, ALL), self.ident_bf)
                self.copy(combT.v(0, ALL, S(g * 1024, (g + 1) * 1024)), V(pk[0:8, 0:1024], bank.v(0).bs))
            if self.stage == l * 10 + 8:
                return self.finish_debug({"comb": (comb, BF16, [128, 16, 8]), "combT": (combT, BF16, [8, SEQ])})
        P.barrier()
        self.slots = self.sb("fslot", [128, 3, 8192], BF16, esf)
        self.nslots = 3
        self.slot_i = 0
        y = self.sb("y", [128, 8, 1024], BF16, esf)
        tf = self.sb("ftf", [128, 2, 512], F32, esf)
        tmpx = self.sb("tmpx", [128, 2, 512], F32, esf)
        cbt = self.sb("cbt", [128, 1024], F32, esf)
        self.cntA = 0
        self.cntB = 0
        nexp = NEXP if getattr(self, "dbg_nexp", None) is None else self.dbg_nexp
        for e in range(nexp):
            def w1_fn(c0, nc_, e=e):
                return w1_d[j, e, :, c0:c0 + nc_].rearrange("(k p) c -> p k c", p=128)

            def w3_fn(c0, nc_, e=e):
                return w3_d[j, e, :, c0:c0 + nc_].rearrange("(k p) c -> p k c", p=128)

            def w2_fn(r0, nr, e=e):
                return w2_d[j, e, r0:r0 + nr, :].rearrange("(k p) c -> p k c", p=128)

            def cb_fn(hi, half, toffs, e=e):
                for ti in half:
                    t0, n = TILES[ti]
                    self.mm(ps[6].v(0, ALL, S(0, n)), self.sel.v(0, ALL, S(e * 128, (e + 1) * 128)),
                            combT.v(0, ALL, S(t0, t0 + n)))
                    self.copy(cbt.v(toffs[ti], ALL, S(toffs[ti], toffs[ti] + n)), ps[6].v(0, ALL, S(0, n)), eng="act")
                return cbt

            self.ffn_expert(w1_fn, w3_fn, w2_fn, D_FFE, [[0, 1], [2, 3]], y, tf, lambda m, s: modv(5, m, s),
                            cb_fn=cb_fn, tmpx=tmpx)
    return None


Builder.moe = _moe


def _moe_sparse(self, l, router_d, ABv, modv):
    P, ps, xT = self.P, self.ps, self.xT
    S, ALL = S_, ALL_
    I32 = mybir.dt.int32
    NTILE = 15
    T = 512
    m1_d = [self.din(f"m1_{b}", [NEXP, D, 256]) for b in range(14)]
    m3_d = [self.din(f"m3_{b}", [NEXP, D, 256]) for b in range(14)]
    m2_d = [[self.din(f"m2_{rp}_{dh}", [NEXP, 512, 512]) for dh in range(2)] for rp in range(7)]
    iota_d = self.din("iota", [128, 512])
    with ExitStack() as esf:
        h2tok = self.sb("h2tok", [128, 16, D], BF16, esf)
        iota_t = self.sb("iota", [128, 512], F32, esf)
        sl1 = self.sb("sl1", [128, 16], F32, esf)
        sl2 = self.sb("sl2", [128, 16], F32, esf)
        w2t = self.sb("w2t", [128, 16], F32, esf)
        dwt = self.sb("dwt", [128, 16], F32, esf)
        esi = self.sb("esi", [128, 16], I32, esf)
        self.dma("sp", iota_t.v(0), DV(iota_d[:, :], [Buf("d")]))
        with ExitStack() as esr:
            wr = self.sb("wr", [128, 8, 8], F32, esr)
            h2f = self.sb("h2f", [128, 8, 512], BF16, esr)
            self.dma("sp", wr.v(0), DV(router_d[l // 2, :, :].rearrange("(k p) e -> p k e", p=128), [Buf("d")]))
            pR = ps[7]

            def dst(c, ti, n):
                return h2f.v(c, ALL, c, S(0, n))

            def after_tile(ti):
                for bi in range(4):
                    bank = ps[2 + (ti * 4 + bi) % 4]
                    pk = bank.t[:, :].bitcast(BF16)
                    for c in range(8):
                        self.tr(V(pk[:, c * 128:(c + 1) * 128], bank.v(0).bs), h2f.v(c, ALL, c, S(bi * 128, (bi + 1) * 128)),
                                self.ident_bf)
                    self.copy(h2tok.v(ti * 4 + bi, ALL, ti * 4 + bi, ALL), V(pk[:, 0:1024], bank.v(0).bs),
                              eng="act" if bi % 2 == 0 else "dve")

            self.norm_mod(1, TILES[:4], ABv, modv, 3, router=(wr, pR), dst=dst, after_tile=after_tile)
            lg = self.sb("lg", [128, 16, 8], F32, esr)
            l2 = self.sb("l2", [128, 16, 8], F32, esr)
            mk1 = self.sb("mk1", [128, 16, 8], F32, esr)
            mk2 = self.sb("mk2", [128, 16, 8], F32, esr)
            Mm = self.sb("Mm", [128, 16, 8], F32, esr)
            rank = self.sb("rank", [128, 16, 8], F32, esr)
            tot = self.sb("tot", [128, 16, 8], F32, esr)
            pbk = self.sb("pbk", [128, 16, 8], F32, esr)
            m1 = self.sb("m1", [128, 16], F32, esr)
            m2 = self.sb("m2", [128, 16], F32, esr)
            w1t = self.sb("w1t", [128, 16], F32, esr)
            triS = self.sb("triS", [128, 128], F32, esr)
            sm8 = self.sb("sm8", [128, 8, 8], F32, esr)
            esf_ = self.sb("esf", [128, 16], F32, esr)

            def bc(tn):
                return V(tn.t[:, :].unsqueeze(2).to_broadcast([128, 16, 8]), tn.v(0).bs)

            def flat(tn):
                return V(tn.t[:, :, :].rearrange("p a b -> p (a b)"), tn.v(0).bs)

            self.copy(flat(lg), pR.v(0, ALL, S(0, 128)))
            self.rmax(m1.v(0), lg.v(0))
            self.tt(mk1.v(0), lg.v(0), bc(m1), ALU.is_equal)
            self.stt(l2.v(0), mk1.v(0), -1e30, lg.v(0), ALU.mult, ALU.add)
            self.rmax(m2.v(0), l2.v(0))
            self.tt(mk2.v(0), l2.v(0), bc(m2), ALU.is_equal)
            self.tt(w2t.v(0), m2.v(0), m1.v(0), ALU.subtract)
            self.act(w2t.v(0), w2t.v(0), AF.Sigmoid)
            self.ts(w1t.v(0), w2t.v(0), -1.0, ALU.mult, 1.0, ALU.add)
            self.tt(dwt.v(0), w1t.v(0), w2t.v(0), ALU.subtract)
            self.tt(Mm.v(0), mk1.v(0), mk2.v(0), ALU.add)
            self.tt(triS.v(0), self.triF, self.ident_f, ALU.subtract)
            self.mm(ps[0].v(0, ALL, S(0, 128)), triS.v(0), flat(Mm))
            self.mm(ps[1].v(0, ALL, S(0, 128)), self.ones_f, flat(Mm))
            self.copy(flat(rank), ps[0].v(0, ALL, S(0, 128)))
            self.copy(flat(tot), ps[1].v(0, ALL, S(0, 128)))
            self.memset(pbk.v(0, ALL, 0, ALL), 0.0)
            for b in range(1, 16):
                self.tt(pbk.v(0, ALL, b, ALL), pbk.v(0, ALL, b - 1, ALL), tot.v(0, ALL, b - 1, ALL), ALU.add)

            def row(i):
                return sm8.v(0, ALL, i, ALL)

            ntot, ntl, st, end, offs, tmp8 = [row(i) for i in range(6)]
            self.tt(ntot, pbk.v(0, ALL, 15, ALL), tot.v(0, ALL, 15, ALL), ALU.add)
            self.ts(ntl, ntot, 0.0, ALU.is_gt)
            for k in range(1, 4):
                self.stt(ntl, ntot, float(T * k), ntl, ALU.is_gt, ALU.add)
            self.memset(sm8.v(0, ALL, 2, S(0, 1)), 0.0)
            for e in range(1, 8):
                self.tt(sm8.v(0, ALL, 2, S(e, e + 1)), sm8.v(0, ALL, 2, S(e - 1, e)), sm8.v(0, ALL, 1, S(e - 1, e)), ALU.add)
            self.tt(end, st, ntl, ALU.add)
            self.ts(offs, st, float(T), ALU.mult)
            self.tt(rank.v(0), rank.v(0), pbk.v(0), ALU.add)
            offs_b = V(sm8.t[:, 4, :].unsqueeze(1).to_broadcast([128, 16, 8]), sm8.v(0).bs)
            self.tt(rank.v(0), rank.v(0), offs_b, ALU.add)
            self.tt(l2.v(0), mk1.v(0), rank.v(0), ALU.mult)
            self.P.add("dve", lambda e: e.reduce_sum(sl1.t[:, :], l2.t[:, :, :], AX.X), reads=[l2.v(0)], writes=[sl1.v(0)])
            self.tt(lg.v(0), mk2.v(0), rank.v(0), ALU.mult)
            self.P.add("dve", lambda e: e.reduce_sum(sl2.t[:, :], lg.t[:, :, :], AX.X), reads=[lg.v(0)], writes=[sl2.v(0)])
            self.memset(esf_.v(0), 0.0)
            for s in range(NTILE):
                self.ts(tmp8, end, float(s), ALU.is_le)
                self.P.add("dve", lambda e, a=esf_.t[:, s:s + 1], b=sm8.t[:, 5, :]: e.reduce_sum(a, b, AX.X),
                           reads=[tmp8], writes=[esf_.v(0)])
            self.ts(esf_.v(0), esf_.v(0), 7.0, ALU.min)
            self.copy(esi.v(0), esf_.v(0))
        P.barrier()
        self.slots = self.sb("fslot", [128, 3, 4096], BF16, esf)
        self.nslots = 3
        self.slot_i = 0
        hs = self.sb("hs", [128, 8, T], BF16, esf)
        y = self.sb("ysp", [128, 28, T], BF16, esf)
        otok = self.sb("otok", [128, 4, D], BF16, esf)
        swt = self.sb("swt", [128, 2, 4, T], BF16, esf)
        a1 = self.sb("a1", [128, 2, T], BF16, esf)
        selb = self.sb("selb", [128, 2, T], BF16, esf)
        tmpw = self.sb("tmpw", [128, 2, T], BF16, esf)
        selw = self.sb("selw", [128, 2, T], BF16, esf)
        tf = self.sb("stf", [128, 2, T], BF16, esf)
        s1s = self.sb("s1s", [128, 2, 16], F32, esf)
        s2s = self.sb("s2s", [128, 2, 16], F32, esf)
        n1s = self.sb("n1s", [128, 2, 16], F32, esf)
        n2s = self.sb("n2s", [128, 2, 16], F32, esf)
        adf = self.sb("adf", [128, 2, T], BF16, esf)
        adf2 = self.sb("adf2", [128, 2, T], BF16, esf)
        w1p = self.sb("w1p", [128, 16], F32, esf)
        w1n = self.sb("w1n", [128, 16], F32, esf)
        w2n = self.sb("w2n", [128, 16], F32, esf)
        self.tt(w1p.v(0), w2t.v(0), dwt.v(0), ALU.add)
        self.ts(w1n.v(0), w1p.v(0), -1.0, ALU.mult)
        self.ts(w2n.v(0), w2t.v(0), -1.0, ALU.mult)
        iota = iota_t.v(0)
        cnt = 0
        ntiles = NTILE if getattr(self, "dbg_ntiles", None) is None else self.dbg_ntiles
        for s in range(ntiles):
            sp_ = s % 2
            holder = {}

            def load_reg(e, s=s, holder=holder):
                r = e.alloc_register(f"ex{s}")
                ins = e.reg_load(r, esi.t[0:1, s:s + 1])
                holder["v"] = e.snap(r, donate=True, min_val=0, max_val=NEXP - 1)
                return ins

            P.add("pool", load_reg, reads=[esi.v(0)])
            self.ts(s1s.v(sp_, ALL, sp_, ALL), sl1.v(0), float(-T * s), ALU.add)
            self.ts(s2s.v(sp_, ALL, sp_, ALL), sl2.v(0), float(-T * s), ALU.add)
            self.ts(n1s.v(sp_, ALL, sp_, ALL), sl1.v(0), -1.0, ALU.mult, float(T * s), ALU.add)
            self.ts(n2s.v(sp_, ALL, sp_, ALL), sl2.v(0), -1.0, ALU.mult, float(T * s), ALU.add)

            def build_sel(tb, dstv, eng="dve"):
                av = a1.v(tb % 2, ALL, tb % 2, ALL)
                dv_ = adf.v(tb % 2, ALL, tb % 2, ALL)
                self.act(dv_, iota, AF.Abs, bias=n1s.v(sp_, ALL, sp_, S(tb, tb + 1)))
                self.act(av, dv_, AF.Relu, bias=self.constcol(1.0), scale=-1.0)
                self.stt(dstv, iota, s2s.v(sp_, ALL, sp_, S(tb, tb + 1)), av, ALU.is_equal, ALU.add)
                return av

            def build_selw(tb, dstv):
                q2 = tb % 2
                d1 = adf.v(q2, ALL, q2, ALL)
                t1 = a1.v(q2, ALL, q2, ALL)
                self.act(d1, iota, AF.Abs, bias=n1s.v(sp_, ALL, sp_, S(tb, tb + 1)))
                self.act(t1, d1, AF.Relu, bias=w1p.v(0, ALL, S(tb, tb + 1)), scale=w1n.v(0, ALL, S(tb, tb + 1)))
                t2 = tmpw.v(q2, ALL, q2, ALL)
                self.ts(t2, iota, s2s.v(sp_, ALL, sp_, S(tb, tb + 1)), ALU.is_equal, w2t.v(0, ALL, S(tb, tb + 1)), ALU.mult)
                self.tt(dstv, t1, t2, ALU.add)

            for tb in range(16):
                sv = selb.v(tb % 2, ALL, tb % 2, ALL)
                build_sel(tb, sv, eng=SEL_ENG)
                for c in range(8):
                    self.mm(ps[c].v(0), h2tok.v(tb, ALL, tb, S(c * 128, (c + 1) * 128)), sv,
                            start=(tb == 0), stop=(tb == 15))
            for c in range(8):
                self.copy(hs.v(c, ALL, c, ALL), ps[c].v(0), eng="act")
            for b in range(14):
                si = self.slot()
                wv = self.slots.t[:, si, 0:4096].rearrange("p (w k c) -> p w k c", w=2, k=8)
                wb = self.slots.v(si).bs

                def dfn(e, b=b, wv=wv, holder=holder):
                    v = holder["v"]
                    i1 = m1_d[b][bass.ds(v, 1), :, :].rearrange("e (k p) c -> p (e k) c", p=128)
                    i3 = m3_d[b][bass.ds(v, 1), :, :].rearrange("e (k p) c -> p (e k) c", p=128)
                    return [e.dma_start(out=wv[:, 0, :, :], in_=i1), e.dma_start(out=wv[:, 1, :, :], in_=i3)]

                P.add("pool", dfn, reads=[DV(None, [Buf("d")])], writes=[V(wv, wb)], dma=2, sembuf=wb[0])
                for jj in range(2):
                    jc = b * 2 + jj
                    cnt += 1
                    par = cnt % 2
                    bU, bV = ps[par], ps[2 + par]
                    for k in range(8):
                        self.mm(bU.v(0), V(wv[:, 0, k, jj * 128:(jj + 1) * 128], wb), hs.v(k, ALL, k, ALL),
                                start=(k == 0), stop=(k == 7))
                    for k in range(8):
                        self.mm(bV.v(0), V(wv[:, 1, k, jj * 128:(jj + 1) * 128], wb), hs.v(k, ALL, k, ALL),
                                start=(k == 0), stop=(k == 7))
                    sv = tf.v(par, ALL, par, ALL)
                    self.act(sv, bU.v(0), AF.Silu)
                    self.tt(y.v(jc, ALL, jc, ALL), sv, bV.v(0), ALU.mult)
            for dh in range(2):
                for rp in range(7):
                    si = self.slot()
                    wv = self.slots.t[:, si, 0:2048].rearrange("p (k c) -> p k c", c=512)
                    wb = self.slots.v(si).bs

                    def dfn2(e, rp=rp, dh=dh, wv=wv, holder=holder):
                        v = holder["v"]
                        i2 = m2_d[rp][dh][bass.ds(v, 1), :, :].rearrange("e (k p) c -> p (e k) c", p=128)
                        return [e.dma_start(out=wv, in_=i2)]

                    P.add("pool", dfn2, reads=[DV(None, [Buf("d")])], writes=[V(wv, wb)], dma=1, sembuf=wb[0])
                    for jj in range(4):
                        jc = rp * 4 + jj
                        for sb in range(4):
                            self.mm(ps[4 + sb].v(0), y.v(jc, ALL, jc, S(sb * 128, (sb + 1) * 128)),
                                    V(wv[:, jj, :], wb), start=(jc == 0), stop=(jc == 27))
                for sb in range(4):
                    self.copy(otok.v((sb, dh), ALL, sb, S(dh * 512, (dh + 1) * 512)), ps[4 + sb].v(0), eng="act")
            def stageX(tt_):
                q2 = tt_ % 2
                banks = [ps[0], ps[1]] if q2 == 0 else [ps[4], ps[5]]
                pks = [bk.t[:, :].bitcast(BF16) for bk in banks]
                for tbi in range(4):
                    tb = tt_ * 4 + tbi
                    swv = selw.v(tb % 2, ALL, tb % 2, ALL)
                    build_selw(tb, swv)
                    for sb in range(4):
                        col = (sb % 2) * 512 + tbi * 128
                        self.tr(V(pks[sb // 2][:, col:col + 128], banks[sb // 2].v(0).bs),
                                selw.v(tb % 2, ALL, tb % 2, S(sb * 128, (sb + 1) * 128)), self.ident_bf)
                for hb in range(2):
                    self.copy(V(swt.t[:, q2, 2 * hb:2 * hb + 2, :].rearrange("p a b -> p (a b)"), swt.v((q2, hb)).bs),
                              V(pks[hb][:, 0:1024], banks[hb].v(0).bs), eng="act")

            def stageY(tt_):
                nonlocal cnt
                q2 = tt_ % 2
                t0 = tt_ * 512
                for c in range(8):
                    cnt += 1
                    bD = ps[2 + cnt % 2]
                    for sb in range(4):
                        self.mm(bD.v(0), otok.v([(sb, 0), (sb, 1)], ALL, sb, S(c * 128, (c + 1) * 128)),
                                swt.v((q2, sb // 2), ALL, q2, sb, ALL), start=(sb == 0), stop=(sb == 3))
                    xv = xT.v((c, tt_), ALL, c, S(t0, t0 + 512))
                    self.stt(xv, bD.v(0), modv(5, c, 0), xv, ALU.mult, ALU.add)

            stageX(0)
            for tt_ in range(4):
                if tt_ + 1 < 4:
                    stageX(tt_ + 1)
                stageY(tt_)
    return None


Builder.moe_sparse = _moe_sparse
```
